# Optimizing a Trainium2 kernel written in Bass

```python
import jax
import jax.numpy as jnp
from jax import lax
import numpy as np

D_MODEL = 4096
BATCH = 8
SEQ = 2048
DEPTH = 1

CTX_LEN = 256
GRID_W = 64
F_GROUPS = 4
F_WIDTH = D_MODEL // 2
F_GROUP_DIM = F_WIDTH // F_GROUPS
NA_HEAD_DIM = 128
NA_WIDTH = D_MODEL // 2
NA_HEADS = NA_WIDTH // NA_HEAD_DIM
NA_ROWS_MAX = 8
NA_COLS = 16
NA_QBLOCK = NA_COLS
NA_REGION_W = 2 * NA_COLS
N_EXPERTS = 16
EXPERT_FF = D_MODEL // 2
EC_FACTOR = 2
N_MOD = 6
RMS_EPS = 1e-6
OFF_Q = F_WIDTH
OFF_K = OFF_Q + NA_WIDTH
OFF_V = OFF_K + NA_WIDTH
OFF_G = OFF_V + NA_WIDTH
IN_WIDTH = OFF_G + 2 * D_MODEL

kernel_name = 'hybrid_fourier_natten_ec_dit'


def rms_norm(x, g):
    xf = x.astype(jnp.float32)
    y = xf * lax.rsqrt(jnp.mean(xf * xf, axis=-1, keepdims=True) + RMS_EPS)
    return (y * g.astype(jnp.float32)).astype(x.dtype)


def ada_params(cond, w_mod, b_mod):
    m = jax.nn.silu(cond) @ w_mod + b_mod
    return jnp.split(m, N_MOD, axis=-1)


def modulate(h, shift, scale):
    return h * (1 + scale) + shift


def split_heads(t):
    b, l, _ = t.shape
    return t.reshape(b, l, NA_HEADS, NA_HEAD_DIM)


def project_in(hn, w_in, b_gate):
    z = hn @ w_in
    u_f, q, k, v, g = jnp.split(z, [OFF_Q, OFF_K, OFF_V, OFF_G], axis=-1)
    g = jax.nn.sigmoid((g + b_gate).astype(jnp.float32)).astype(hn.dtype)
    g_fourier, g_na = jnp.split(g, 2, axis=-1)
    return u_f, split_heads(q), split_heads(k), split_heads(v), g_fourier, g_na


def fourier_mix(u):
    b, l, _ = u.shape
    ug = u.reshape(b, l, F_GROUPS, F_GROUP_DIM).astype(jnp.float32)
    z = jnp.fft.fftn(ug, axes=(1, 3), norm='ortho')
    return jnp.real(z).reshape(b, l, F_WIDTH).astype(u.dtype)


def neighbourhood_attention(q, k, v, k_ctx, v_ctx, rel_bias):
    b, l, h, dh = q.shape
    rows = l // GRID_W
    kr = min(NA_ROWS_MAX, rows)
    n_cb = GRID_W // NA_QBLOCK
    scale = dh ** -0.5
    q_cols = np.arange(GRID_W).reshape(n_cb, NA_QBLOCK)
    reg_start = np.clip(q_cols[:, 0] - NA_COLS // 2, 0, GRID_W - NA_REGION_W)
    key_cols = reg_start[:, None] + np.arange(NA_REGION_W)[None, :]
    win_start = np.clip(q_cols - NA_COLS // 2, 0, GRID_W - NA_COLS)
    rel_col = key_cols[:, None, :] - win_start[:, :, None]
    col_mask = (rel_col >= 0) & (rel_col < NA_COLS)
    dc_idx = np.clip(key_cols[:, None, :] - q_cols[:, :, None] + NA_COLS - 1, 0, 2 * NA_COLS - 2)
    bias_c = rel_bias.astype(jnp.float32)[:, :, dc_idx]
    neg = jnp.finfo(jnp.float32).min

    kf = k.reshape(b, rows, GRID_W, h, dh)
    vf = v.reshape(b, rows, GRID_W, h, dh)
    q_rows = jnp.moveaxis(q.reshape(b, rows, n_cb, NA_QBLOCK, h, dh), 1, 0)
    n_loc = kr * NA_REGION_W

    def one_row(args):
        r, qr = args
        rs = jnp.clip(r - kr // 2, 0, rows - kr)
        kw = lax.dynamic_slice_in_dim(kf, rs, kr, axis=1)[:, :, key_cols]
        vw = lax.dynamic_slice_in_dim(vf, rs, kr, axis=1)[:, :, key_cols]
        s_loc = jnp.einsum('bcqhd,bicjhd->bhcqij', qr, kw).astype(jnp.float32) * scale
        dr_idx = rs + jnp.arange(kr) - r + NA_ROWS_MAX - 1
        bias = jnp.transpose(jnp.take(bias_c, dr_idx, axis=1), (0, 2, 3, 1, 4))
        s_loc = jnp.where(col_mask[:, :, None, :], s_loc + bias[None], neg)
        s_loc = s_loc.reshape(b, h, n_cb, NA_QBLOCK, n_loc)
        s_ctx = jnp.einsum('bcqhd,bjhd->bhcqj', qr, k_ctx).astype(jnp.float32) * scale
        p = jax.nn.softmax(jnp.concatenate([s_loc, s_ctx], axis=-1), axis=-1).astype(v.dtype)
        p_loc = p[..., :n_loc].reshape(b, h, n_cb, NA_QBLOCK, kr, NA_REGION_W)
        p_ctx = p[..., n_loc:]
        return (jnp.einsum('bhcqij,bicjhd->bcqhd', p_loc, vw)
                + jnp.einsum('bhcqj,bjhd->bcqhd', p_ctx, v_ctx))

    o = lax.map(one_row, (jnp.arange(rows), q_rows))
    return jnp.moveaxis(o, 0, 1).reshape(b, l, h * dh)


def context_attention(q, k, v):
    b, l, h, dh = q.shape
    s = jnp.einsum('blhd,bjhd->bhlj', q, k).astype(jnp.float32) * dh ** -0.5
    p = jax.nn.softmax(s, axis=-1).astype(v.dtype)
    return jnp.einsum('bhlj,bjhd->blhd', p, v).reshape(b, l, h * dh)


def merge_branches(u_f, o_na, g_fourier, g_na, w_fourier, w_na_out, w_out):
    y_f = fourier_mix(u_f) @ w_fourier
    y_n = o_na @ w_na_out
    return (g_fourier * y_f + g_na * y_n) @ w_out


def expert_choice_ffn(h, w_router, w1, w3, w2):
    b, n, _ = h.shape
    cap = EC_FACTOR * n // N_EXPERTS
    aff = jax.nn.softmax((h @ w_router).astype(jnp.float32), axis=-1)
    g, idx = lax.top_k(jnp.swapaxes(aff, 1, 2), cap)
    bidx = jnp.arange(b)[:, None, None]
    xin = h[bidx, idx]
    hid = jax.nn.silu(jnp.einsum('becd,edf->becf', xin, w1)) * jnp.einsum('becd,edf->becf', xin, w3)
    out = jnp.einsum('becf,efd->becd', hid, w2) * g[..., None].astype(h.dtype)
    return jnp.zeros_like(h).at[bidx, idx].add(out)


def setup_inputs(seed: int = 0) -> dict:
    key = jax.random.key(seed)
    ks = jax.random.split(key, 20)
    L = DEPTH

    def nrm(k, shape, scale):
        return jax.random.normal(k, shape, jnp.float32) * scale

    return {
        'x': nrm(ks[0], (BATCH, SEQ, D_MODEL), 1.0),
        'c': nrm(ks[1], (BATCH, D_MODEL), 1.0),
        'ctx': nrm(ks[2], (BATCH, CTX_LEN, D_MODEL), 1.0),
        'c_ctx': nrm(ks[3], (D_MODEL,), 1.0),
        'w_mod': nrm(ks[4], (L, D_MODEL, N_MOD * D_MODEL), 0.5 * D_MODEL ** -0.5),
        'b_mod': nrm(ks[5], (L, N_MOD * D_MODEL), 0.01),
        'norm_mix_g': 1.0 + nrm(ks[6], (L, D_MODEL), 0.01),
        'w_in': nrm(ks[7], (L, D_MODEL, IN_WIDTH), D_MODEL ** -0.5),
        'b_gate': nrm(ks[8], (L, 2 * D_MODEL), 0.01),
        'w_fourier': nrm(ks[9], (L, F_WIDTH, D_MODEL), F_WIDTH ** -0.5),
        'na_rel_bias': nrm(ks[10], (L, NA_HEADS, 2 * NA_ROWS_MAX - 1, 2 * NA_COLS - 1), 0.1),
        'w_na_out': nrm(ks[11], (L, NA_WIDTH, D_MODEL), NA_WIDTH ** -0.5),
        'w_out': nrm(ks[12], (L, D_MODEL, D_MODEL), D_MODEL ** -0.5),
        'norm_ffn_g': 1.0 + nrm(ks[13], (L, D_MODEL), 0.01),
        'w_router': nrm(ks[14], (L, D_MODEL, N_EXPERTS), D_MODEL ** -0.5),
        'w1': nrm(ks[15], (L, N_EXPERTS, D_MODEL, EXPERT_FF), D_MODEL ** -0.5),
        'w3': nrm(ks[16], (L, N_EXPERTS, D_MODEL, EXPERT_FF), D_MODEL ** -0.5),
        'w2': nrm(ks[17], (L, N_EXPERTS, EXPERT_FF, D_MODEL), EXPERT_FF ** -0.5),
        'final_norm_g': 1.0 + nrm(ks[18], (D_MODEL,), 0.01),
    }


def reference(x, c, ctx, c_ctx, w_mod, b_mod, norm_mix_g, w_in, b_gate, w_fourier, na_rel_bias,
              w_na_out, w_out, norm_ffn_g, w_router, w1, w3, w2, final_norm_g):
    for layer in range(DEPTH):
        last = layer == DEPTH - 1
        sh_m, sc_m, gt_m, sh_f, sc_f, gt_f = [t[:, None, :] for t in ada_params(c, w_mod[layer], b_mod[layer])]
        csh_m, csc_m, cgt_m, csh_f, csc_f, cgt_f = ada_params(c_ctx, w_mod[layer], b_mod[layer])

        xn = modulate(rms_norm(x, norm_mix_g[layer]), sh_m, sc_m)
        cn = modulate(rms_norm(ctx, norm_mix_g[layer]), csh_m, csc_m)
        u_f, q, k, v, g_fr, g_na = project_in(xn, w_in[layer], b_gate[layer])
        if last:
            k_c, v_c = [split_heads(t) for t in jnp.split(cn @ w_in[layer][:, OFF_K:OFF_G], 2, axis=-1)]
        else:
            u_fc, q_c, k_c, v_c, g_frc, g_nac = project_in(cn, w_in[layer], b_gate[layer])
        o_na = neighbourhood_attention(q, k, v, k_c, v_c, na_rel_bias[layer])
        x = x + gt_m * merge_branches(u_f, o_na, g_fr, g_na, w_fourier[layer], w_na_out[layer], w_out[layer])

        xn = modulate(rms_norm(x, norm_ffn_g[layer]), sh_f, sc_f)
        x = x + gt_f * expert_choice_ffn(xn, w_router[layer], w1[layer], w3[layer], w2[layer])

        if not last:
            o_c = context_attention(q_c, k_c, v_c)
            ctx = ctx + cgt_m * merge_branches(u_fc, o_c, g_frc, g_nac, w_fourier[layer], w_na_out[layer], w_out[layer])
            cn = modulate(rms_norm(ctx, norm_ffn_g[layer]), csh_f, csc_f)
            ctx = ctx + cgt_f * expert_choice_ffn(cn, w_router[layer], w1[layer], w3[layer], w2[layer])
    return rms_norm(x, final_norm_g)
```

```python
import contextlib
import numpy as np
import concourse.bass as bass
import concourse.mybir as mybir
from concourse.bass_utils import run_bass_kernel_spmd

F32 = mybir.dt.float32
F32R = mybir.dt.float32r
AF = mybir.ActivationFunctionType
ALU = mybir.AluOpType
AXL = mybir.AxisListType

D = 4096
T = 2048
CT = 256
NE = 16
CAP = 256
FF = 2048
NH_ = 16
EPS = 1e-6
NEG = -200.0
SCALE = 128 ** -0.5

NL = 4
LSLOT = 4096
NS = 6
NA = 8


class Sem:
    def __init__(self, nc, es, name):
        self.h = es.enter_context(nc.semaphore(name))
        self.n = 0


class StopBuild(Exception):
    pass


class K:
    def __init__(self, debug=(), stop=None):
        self.debug = set(debug)
        self.stop = stop
        self.nphase = 0
        nc = bass.Bass("TRN2", target_bir_lowering=False)
        self.nc = nc
        self.es = contextlib.ExitStack()
        self.PE, self.ACT, self.DVE, self.POOL, self.SP = nc.tensor, nc.scalar, nc.vector, nc.gpsimd, nc.sync
        self.engs = [self.PE, self.ACT, self.DVE, self.POOL, self.SP]
        self.pending = {}
        self.nsem = 0

    def sem(self, name):
        self.nsem += 1
        return Sem(self.nc, self.es, name)

    def din(self, name, shape):
        return self.nc.dram_tensor(name, list(shape), F32, kind="ExternalInput").ap()

    def dscr(self, name, shape):
        kind = "ExternalOutput" if name in self.debug else "Internal"
        return self.nc.dram_tensor(name, list(shape), F32, kind=kind).ap()

    def sb(self, name, shape, dt=F32):
        return self.es.enter_context(self.nc.sbuf_tensor("sb_" + name, list(shape), dt))

    def wait(self, eng, sem, val):
        if val > 0:
            eng.wait_ge(sem.h, val)

    def inc(self, instr, sem, k=1):
        instr.then_inc(sem.h, k)
        sem.n += k
        return sem.n

    def note_store(self, sem):
        self.pending[id(sem)] = sem

    def phase_end(self, light=False):
        for s in self.pending.values():
            for e in ([self.POOL] if light else self.engs):
                self.wait(e, s, s.n)
        if not light:
            self.pending = {}
        self.nphase += 1
        if self.stop is not None and self.nphase >= self.stop:
            raise StopBuild()


def build(debug=(), stop=None):
    g = K(debug, stop)
    with g.es:
        try:
            _body(g)
        except StopBuild:
            pass
    return g


def _body(g):
    nc, es = g.nc, g.es
    PE, ACT, DVE, POOL, SP = g.PE, g.ACT, g.DVE, g.POOL, g.SP

    def ev(instr, sem, k=1):
        instr.then_inc(sem.h, k)
        sem.n += k
        return (sem, sem.n)

    def waitp(eng, pairs):
        if pairs is None:
            return
        if isinstance(pairs, tuple):
            pairs = [pairs]
        for (s_, v_) in pairs:
            if v_ > 0:
                eng.wait_ge(s_.h, v_)

    x_d = g.din("x", [T, D])
    ctx_d = g.din("ctx", [CT, D])
    c2T_d = g.din("c2T", [128, 64])
    bmodT_d = g.din("bmodT", [128, 192])
    gvec_d = g.din("gvec", [128, 160])
    cst_d = g.din("cst", [128, 768])
    wmod_d = g.din("w_mod", [D, 6 * D])
    win_d = g.din("w_in", [D, 4 * D])
    wfo_d = g.din("w_fourier", [2048, D])
    wna_d = g.din("w_na_out", [2048, D])
    wout_d = g.din("w_out", [D, D])
    wr_d = g.din("w_router", [D, NE])
    w1_d = g.din("w1", [NE, D, FF])
    w3_d = g.din("w3", [NE, D, FF])
    w2_d = g.din("w2", [NE, FF, D])
    cs_d = g.din("dft_cs", [512, 1024])
    cls_d = g.din("dft_cls", [2 * T, T])
    nab_d = g.din("nab", [NH_, 128, 14 * 64])
    out_d = nc.dram_tensor("out", [T, D], F32, kind="ExternalOutput").ap()

    S_xT = g.dscr("S_xT", [D, T])
    S_xnT = g.dscr("S_xnT", [D, T])
    S_cnT = g.dscr("S_cnT", [D, CT])
    S_ufT = g.dscr("S_ufT", [2048, T])
    S_qT = g.dscr("S_qT", [2048, T])
    S_kT = g.dscr("S_kT", [2048, T])
    S_v = g.dscr("S_v", [T, 2048])
    S_gT = g.dscr("S_gT", [2 * D, T])
    S_kcT = g.dscr("S_kcT", [2048, CT])
    S_vc = g.dscr("S_vc", [CT, 2048])
    S_AB = g.dscr("S_AB", [4, T, 1024])
    S_YT = g.dscr("S_YT", [2048, T])
    S_onT = g.dscr("S_onT", [2048, T])
    S_mT = g.dscr("S_mT", [D, T])
    S_x1T = g.dscr("S_x1T", [D, T])
    S_xn2T = g.dscr("S_xn2T", [D, T])
    S_xn2 = g.dscr("S_xn2", [T, D])
    S_selgT = g.dscr("S_selgT", [NE * CAP, T])
    S_eo = g.dscr("S_eo", [NE * CAP, D])
    S_x2T = g.dscr("S_x2T", [D, T])

    RB = g.sb("RB", [128, 16384], F32R)
    LR = [g.sb(f"LR{i}", [128, LSLOT], F32R) for i in range(NL)]
    FB = g.sb("FB", [128, 16384], F32)
    cst = g.sb("cst", [128, 768], F32)
    onesR = g.sb("onesR", [128, 128], F32R)
    tokR = g.sb("tokR", [128, 32], F32R)
    IDX = g.sb("idx", [128, 4], mybir.dt.int32)
    modT = g.sb("modT", [128, 192 * 2], F32)
    bmodT = g.sb("bmodT", [128, 192], F32)
    gvec = g.sb("gvec", [128, 160], F32)
    vec = g.sb("vec", [128, 10 * 32], F32)
    sT = g.sb("sT", [128, 64], F32R)
    c2T = g.sb("c2T", [128, 64], F32)
    small = g.sb("small", [128, 64], F32)
    rt = g.sb("rt", [128, 6 * 256], F32)
    rsm = g.sb("rsm", [128, 64], F32)
    affT = FB[0:16, 0:2 * T + 64]
    PS = es.enter_context(nc.psum_tensor("PS", [128, 8, 512], F32))

    ident = cst[:, 0:128]
    ones = cst[:, 128:256]
    tri = cst[:, 256:384]
    iota = cst[:, 384:640]

    VEC = {n: vec[:, i * 32:(i + 1) * 32] for i, n in enumerate(
        ["gs_m", "sh_m", "gs_mc", "sh_mc", "gt_m", "gs_f", "sh_f", "gt_f", "g_fin", "zero"])}
    bgate = gvec[:, 96:160]

    l_loaded = [g.sem(f"l_ld{i}") for i in range(NL)]
    l_free = [g.sem(f"l_fr{i}") for i in range(NL)]
    rb_loaded = g.sem("rb_ld")
    ps_free = [g.sem("ps_fr0"), g.sem("ps_fr1")]
    st_free = [g.sem(f"st_fr{i}") for i in range(NS)]
    ax_loaded = [[g.sem(f"ax_ld{k}_{i}") for i in range(NA)] for k in range(2)]
    s_misc = g.sem("misc")
    s_dve = g.sem("dve_ev")
    s_dve2 = g.sem("dve_ev2")
    s_act = g.sem("act_ev")
    s_pe = g.sem("pe_ev")
    s_x = [g.sem("x_ld0"), g.sem("x_ld1")]

    st = {"li": 0, "si": 0, "ai": [0, 0], "pset": 0, "cp": 0, "rb_busy": None}
    l_busy = [None] * NL
    ps_busy = [[], []]
    st_busy = [None] * NS
    ax_busy = [[None] * NA for _ in range(2)]
    STG = [FB[:, 12288 + i * 512: 12288 + (i + 1) * 512] for i in range(NS)]
    AUX = [[FB[:, 4096 + (k * NA + i) * 512: 4096 + (k * NA + i + 1) * 512] for i in range(NA)] for k in range(2)]

    def stage_acquire(eng):
        i = st["si"]; st["si"] += 1
        slot = i % NS
        waitp(eng, st_busy[slot])
        return slot

    def stage_store(slot, ready, dst_ap, src_ap=None):
        waitp(SP, ready)
        d = SP.dma_start(out=dst_ap, in_=STG[slot] if src_ap is None else src_ap)
        st_busy[slot] = ev(d, st_free[slot], 16)
        g.note_store(st_free[slot])

    def aux_load(kind, src_ap, w=512):
        i = st["ai"][kind]; st["ai"][kind] += 1
        slot = i % NA
        waitp(POOL, ax_busy[kind][slot])
        d = POOL.dma_start(out=AUX[kind][slot][:, 0:w], in_=src_ap)
        return (kind, slot, ev(d, ax_loaded[kind][slot], 16))

    def aux_use(eng, tok):
        waitp(eng, tok[2])
        return AUX[tok[0]][tok[1]]

    def aux_release(tok, pair):
        ax_busy[tok[0]][tok[1]] = pair

    def copy_eng():
        st["cp"] += 1
        return ACT if st["cp"] % 2 == 0 else DVE

    def evac_copy(eng, out, in_):
        if eng is ACT:
            return ACT.activation(out=out, in_=in_, func=AF.Copy)
        return DVE.tensor_copy(out=out, in_=in_)

    def ps_acquire():
        pset = st["pset"] % 2
        st["pset"] += 1
        waitp(PE, ps_busy[pset])
        ps_busy[pset] = []
        return pset

    def ps_rel(pset):
        def rel(instr):
            p = ev(instr, ps_free[pset])
            ps_busy[pset].append(p)
            return p
        return rel

    def ps_all():
        return list(ps_busy[0]) + list(ps_busy[1])

    def gemm(Kd, M, N, Lsrc, bank, Rsrc=None, Rres=None, MW=512, Nb=512, prep=None, r_wait=None):
        KT = Kd // 128
        KC = min(KT, LSLOT // MW)
        nkt = KT // KC
        W = min(512, Nb)
        NHh = max(1, Nb // 512)
        MC = MW // 128
        nb_banks = MC * NHh
        assert nb_banks <= 4 and KT % KC == 0 and M % MW == 0 and N % Nb == 0
        RBv = RB[:, 0:KT * Nb].rearrange("p (k n) -> p k n", k=KT) if Rres is None else None
        last = None
        for nb in range(N // Nb):
            n0 = nb * Nb
            rb_ready = None
            if Rres is None:
                waitp(POOL, st["rb_busy"])
                RKC = max(1, min(KT, 4096 // Nb))
                for kk in range(0, KT, RKC):
                    d = POOL.dma_start(out=RBv[:, kk:kk + RKC, :],
                                       in_=Rsrc(kk, RKC, n0, Nb).rearrange("(kc p) n -> p kc n", p=128))
                    rb_ready = ev(d, rb_loaded, 16)
            first = True
            for mg in range(M // MW):
                m0 = mg * MW
                ctx = prep(m0, n0) if prep is not None else None
                pset = ps_acquire()
                if first:
                    waitp(PE, rb_ready)
                    waitp(PE, r_wait)
                    first = False
                for kt in range(nkt):
                    i = st["li"]; st["li"] += 1
                    slot = i % NL
                    waitp(POOL, l_busy[slot])
                    Lt = LR[slot][:, 0:KC * MW].rearrange("p (k m) -> p k m", k=KC)
                    d = POOL.dma_start(out=Lt, in_=Lsrc(kt * KC, KC, m0, MW).rearrange("(kc p) m -> p kc m", p=128))
                    waitp(PE, ev(d, l_loaded[slot], 16))
                    mm = None
                    for kc in range(KC):
                        k = kt * KC + kc
                        for mc in range(MC):
                            for nh in range(NHh):
                                rhs = RBv[:, k, nh * W:(nh + 1) * W] if Rres is None else Rres(k, nh * W, W)
                                mm = PE.matmul(PS[:, pset * 4 + mc * NHh + nh, 0:W],
                                               lhsT=Lt[:, kc, mc * 128:(mc + 1) * 128], rhs=rhs,
                                               start=(k == 0), stop=(k == KT - 1))
                    last = ev(mm, l_free[slot])
                    l_busy[slot] = last
                if Rres is None:
                    st["rb_busy"] = last
                rel = ps_rel(pset)
                for mc in range(MC):
                    for nh in range(NHh):
                        bank(PS[:, pset * 4 + mc * NHh + nh, 0:W], m0 + mc * 128, n0 + nh * W, W, last, rel, ctx)
        return last

    def gemm_sr(Kd, M, N, Lres, Rsrc, bank, l_wait=None):
        KT = Kd // 128
        NW = 512
        KC = min(KT, LSLOT // NW)
        nkt = KT // KC
        MC = M // 128
        assert MC <= 4 and KT % KC == 0 and N % NW == 0
        first = True
        last = None
        for ng in range(N // NW):
            n0 = ng * NW
            pset = ps_acquire()
            if first:
                waitp(PE, l_wait)
                first = False
            for kt in range(nkt):
                i = st["li"]; st["li"] += 1
                slot = i % NL
                waitp(POOL, l_busy[slot])
                Rt = LR[slot][:, 0:KC * NW].rearrange("p (k n) -> p k n", k=KC)
                d = POOL.dma_start(out=Rt, in_=Rsrc(kt * KC, KC, n0, NW).rearrange("(kc p) n -> p kc n", p=128))
                waitp(PE, ev(d, l_loaded[slot], 16))
                mm = None
                for kc in range(KC):
                    k = kt * KC + kc
                    for mc in range(MC):
                        mm = PE.matmul(PS[:, pset * 4 + mc, 0:NW], lhsT=Lres(k, mc), rhs=Rt[:, kc, :],
                                       start=(k == 0), stop=(k == KT - 1))
                last = ev(mm, l_free[slot])
                l_busy[slot] = last
            rel = ps_rel(pset)
            for mc in range(MC):
                bank(PS[:, pset * 4 + mc, 0:NW], mc * 128, n0, NW, last, rel, None)
        return last

    def bank_copy_to(dst_fn):
        def bank(ps, mrow, n0, w, pe_wait, rel, ctx):
            eng = copy_eng()
            waitp(eng, pe_wait)
            slot = stage_acquire(eng)
            i = evac_copy(eng, STG[slot][:, 0:w], ps)
            stage_store(slot, rel(i), dst_fn(mrow, n0, w), STG[slot][:, 0:w])
        return bank

    ld = None
    for (dst, src) in [(cst, cst_d), (bmodT, bmodT_d), (gvec, gvec_d), (c2T, c2T_d)]:
        ld = ev(SP.dma_start(out=dst[:], in_=src), s_misc, 16)
    for e_ in (ACT, DVE, PE):
        waitp(e_, ld)
    ev(ACT.activation(out=sT[:], in_=c2T[:], func=AF.Silu), s_act)
    ev(ACT.activation(out=onesR[:], in_=ones, func=AF.Copy), s_act)
    sT_ready = ev(ACT.activation(out=tokR[:], in_=cst[:, 640:672], func=AF.Copy), s_act)

    modT3 = modT[:].rearrange("p (n j) -> p n j", j=2)

    def bank_mod(ps, mrow, n0, w, pe_wait, rel, ctx):
        waitp(DVE, pe_wait)
        ch = mrow // 128
        rel(DVE.tensor_scalar(out=modT3[:, ch, :], in0=ps, scalar1=bmodT[:, ch:ch + 1], scalar2=None, op0=ALU.add))

    gemm(D, 6 * D, 2, lambda k0, kn, m0, mw: wmod_d[k0 * 128:(k0 + kn) * 128, m0:m0 + mw], bank_mod,
         Rres=lambda k, n0, w: sT[:, 2 * k:2 * k + 2], MW=512, Nb=2, r_wait=sT_ready)
    waitp(DVE, ps_all())

    def mv(which, j):
        return modT3[:, which * 32:(which + 1) * 32, j]
    ops = [
        ("gs_m", lambda o: DVE.scalar_tensor_tensor(out=o, in0=mv(1, 0), scalar=1.0, in1=gvec[:, 0:32], op0=ALU.add, op1=ALU.mult)),
        ("sh_m", lambda o: DVE.tensor_copy(out=o, in_=mv(0, 0))),
        ("gs_mc", lambda o: DVE.scalar_tensor_tensor(out=o, in0=mv(1, 1), scalar=1.0, in1=gvec[:, 0:32], op0=ALU.add, op1=ALU.mult)),
        ("sh_mc", lambda o: DVE.tensor_copy(out=o, in_=mv(0, 1))),
        ("gt_m", lambda o: DVE.tensor_copy(out=o, in_=mv(2, 0))),
        ("gs_f", lambda o: DVE.scalar_tensor_tensor(out=o, in0=mv(4, 0), scalar=1.0, in1=gvec[:, 32:64], op0=ALU.add, op1=ALU.mult)),
        ("sh_f", lambda o: DVE.tensor_copy(out=o, in_=mv(3, 0))),
        ("gt_f", lambda o: DVE.tensor_copy(out=o, in_=mv(5, 0))),
        ("g_fin", lambda o: DVE.tensor_copy(out=o, in_=gvec[:, 64:96])),
    ]
    for n_, f in ops:
        ev(f(VEC[n_]), s_dve)
    vec_ready = ev(DVE.memset(VEC["zero"], 0.0), s_dve)
    for e_ in (ACT, PE, POOL, SP, DVE):
        waitp(e_, vec_ready)

    XT = [FB[:, 0:4096], FB[:, 4096:8192]]
    XS = FB[:, 8192:12288]
    n_tiles = T // 128 + CT // 128
    xt_busy = [[], []]
    xs_busy = None
    for it in range(n_tiles):
        is_ctx = it >= T // 128
        src = ctx_d[(it - 16) * 128:(it - 15) * 128, :] if is_ctx else x_d[it * 128:(it + 1) * 128, :]
        xs_ = it % 2
        waitp(SP, xt_busy[xs_])
        xt_busy[xs_] = []
        x_ready = ev(SP.dma_start(out=XT[xs_], in_=src), s_x[xs_], 16)
        waitp(ACT, x_ready)
        waitp(ACT, xs_busy)
        sq = ev(ACT.activation(out=XS, in_=XT[xs_], func=AF.Square, accum_out=small[:, it:it + 1]), s_act)
        waitp(DVE, sq)
        p1 = ev(DVE.tensor_scalar(out=small[:, 32 + it:33 + it], in0=small[:, it:it + 1], scalar1=1.0 / D, scalar2=EPS, op0=ALU.mult, op1=ALU.add), s_dve)
        waitp(ACT, p1)
        pq = ev(ACT.activation(out=small[:, 32 + it:33 + it], in_=small[:, 32 + it:33 + it], func=AF.Sqrt), s_act)
        waitp(DVE, pq)
        p2 = ev(DVE.reciprocal(out=small[:, 32 + it:33 + it], in_=small[:, 32 + it:33 + it]), s_dve)
        waitp(DVE, p2)
        xs_ready = ev(DVE.tensor_scalar(out=XS, in0=XT[xs_], scalar1=small[:, 32 + it:33 + it], scalar2=None, op0=ALU.mult), s_dve)
        xt_busy[xs_].append(xs_ready)
        gs = VEC["gs_mc"] if is_ctx else VEC["gs_m"]
        sh = VEC["sh_mc"] if is_ctx else VEC["sh_m"]
        waitp(PE, x_ready)
        for kind in (0, 1):
            if kind == 0 and is_ctx:
                continue
            srcT = XT[xs_] if kind == 0 else XS
            if kind == 1:
                waitp(PE, xs_ready)
            for gq in range(8):
                pset = ps_acquire()
                tr = None
                for q in range(4):
                    c = gq * 4 + q
                    tr = PE.transpose(PS[:, pset * 4, q * 128:(q + 1) * 128], srcT[:, c * 128:(c + 1) * 128], ident)
                pe_done = ev(tr, s_pe)
                if gq == 7:
                    if kind == 0:
                        xt_busy[xs_].append(pe_done)
                    else:
                        xs_busy = pe_done
                eng = ACT if kind == 1 else copy_eng()
                waitp(eng, pe_done)
                slot = stage_acquire(eng)
                lasti = None
                if kind == 0:
                    lasti = evac_copy(eng, STG[slot], PS[:, pset * 4, :])
                    dst = S_xT[gq * 512:(gq + 1) * 512, it * 128:(it + 1) * 128]
                else:
                    for q in range(4):
                        c = gq * 4 + q
                        lasti = ACT.activation(out=STG[slot][:, q * 128:(q + 1) * 128], in_=PS[:, pset * 4, q * 128:(q + 1) * 128],
                                               func=AF.Identity, bias=sh[:, c:c + 1], scale=gs[:, c:c + 1])
                    if is_ctx:
                        dst = S_cnT[gq * 512:(gq + 1) * 512, (it - 16) * 128:(it - 15) * 128]
                    else:
                        dst = S_xnT[gq * 512:(gq + 1) * 512, it * 128:(it + 1) * 128]
                rdy = ps_rel(pset)(lasti)
                stage_store(slot, rdy, dst.rearrange("(q p) t -> p q t", p=128), STG[slot].rearrange("p (q t) -> p q t", q=4))
    g.phase_end()

    def win_dst(mrow, n0, w):
        if mrow < 2048:
            return S_ufT[mrow:mrow + 128, n0:n0 + w]
        if mrow < 4096:
            return S_qT[mrow - 2048:mrow - 2048 + 128, n0:n0 + w]
        return S_kT[mrow - 4096:mrow - 4096 + 128, n0:n0 + w]
    copy_win = bank_copy_to(win_dst)

    def bank_win(ps, mrow_, n0, w, pe_wait, rel, ctx):
        mrow = mrow_ if mrow_ < 6144 else mrow_ + 2048
        if mrow < 6144:
            return copy_win(ps, mrow, n0, w, pe_wait, rel, ctx)
        gi = (mrow - 8192) // 128
        waitp(ACT, pe_wait)
        slot = stage_acquire(ACT)
        i = ACT.activation(out=STG[slot][:, 0:w], in_=ps, func=AF.Sigmoid, bias=bgate[:, gi:gi + 1], scale=1.0)
        stage_store(slot, rel(i), S_gT[mrow - 8192:mrow - 8192 + 128, n0:n0 + w], STG[slot][:, 0:w])

    def win_L(k0, kn, m0, mw):
        col = m0 if m0 < 6144 else m0 + 2048
        return win_d[k0 * 128:(k0 + kn) * 128, col:col + mw]

    gemm(D, 6144 + 8192, T, win_L, bank_win,
         Rsrc=lambda k0, kn, n0, nw: S_xnT[k0 * 128:(k0 + kn) * 128, n0:n0 + nw], MW=512, Nb=512)
    gemm(D, T, 2048, lambda k0, kn, m0, mw: S_xnT[k0 * 128:(k0 + kn) * 128, m0:m0 + mw],
         bank_copy_to(lambda mrow, n0, w: S_v[mrow:mrow + 128, n0:n0 + w]),
         Rsrc=lambda k0, kn, n0, nw: win_d[k0 * 128:(k0 + kn) * 128, 6144 + n0:6144 + n0 + nw], MW=512, Nb=512)
    gemm(D, 2048, CT, lambda k0, kn, m0, mw: win_d[k0 * 128:(k0 + kn) * 128, 4096 + m0:4096 + m0 + mw],
         bank_copy_to(lambda mrow, n0, w: S_kcT[mrow:mrow + 128, n0:n0 + w]),
         Rsrc=lambda k0, kn, n0, nw: S_cnT[k0 * 128:(k0 + kn) * 128, n0:n0 + nw], MW=512, Nb=256)
    gemm(D, CT, 2048, lambda k0, kn, m0, mw: S_cnT[k0 * 128:(k0 + kn) * 128, m0:m0 + mw],
         bank_copy_to(lambda mrow, n0, w: S_vc[mrow:mrow + 128, n0:n0 + w]),
         Rsrc=lambda k0, kn, n0, nw: win_d[k0 * 128:(k0 + kn) * 128, 6144 + n0:6144 + n0 + nw], MW=256, Nb=512)
    g.phase_end(light=True)

    for gi in range(4):
        gemm(512, T, 1024, lambda k0, kn, m0, mw, gi=gi: S_ufT[gi * 512 + k0 * 128: gi * 512 + (k0 + kn) * 128, m0:m0 + mw],
             bank_copy_to(lambda mrow, n0, w, gi=gi: S_AB[gi, mrow:mrow + 128, n0:n0 + w]),
             Rsrc=lambda k0, kn, n0, nw: cs_d[k0 * 128:(k0 + kn) * 128, n0:n0 + nw], MW=256, Nb=1024)
    g.phase_end(light=True)
    def L_ab(k0, kn, m0, mw):
        gi, mo = m0 // 512, m0 % 512
        if k0 < 16:
            return S_AB[gi, k0 * 128:(k0 + kn) * 128, mo:mo + mw]
        return S_AB[gi, (k0 - 16) * 128:(k0 - 16 + kn) * 128, 512 + mo:512 + mo + mw]
    gemm(2 * T, 2048, T, L_ab,
         bank_copy_to(lambda mrow, n0, w: S_YT[mrow:mrow + 128, n0:n0 + w]),
         Rsrc=lambda k0, kn, n0, nw: cls_d[k0 * 128:(k0 + kn) * 128, n0:n0 + nw], MW=512, Nb=512)
    g.phase_end(light=True)

    def prep_aux(srcs, MC=4, NHh=1):
        def prep(m0, n0):
            toks = {}
            for mc in range(MC):
                for nh in range(NHh):
                    toks[(m0 + mc * 128, n0 + nh * 512)] = [
                        aux_load(k, s_[off + m0 + mc * 128: off + m0 + (mc + 1) * 128, n0 + nh * 512:n0 + (nh + 1) * 512])
                        for k, (s_, off) in enumerate(srcs)]
            return toks
        return prep

    def bank_mul_gate(ps, mrow, n0, w, pe_wait, rel, ctx):
        tk = ctx[(mrow, n0)][0]
        waitp(DVE, pe_wait)
        a = aux_use(DVE, tk)
        slot = stage_acquire(DVE)
        p = rel(DVE.tensor_tensor(out=STG[slot][:, 0:w], in0=ps, in1=a[:, 0:w], op=ALU.mult))
        aux_release(tk, p)
        stage_store(slot, p, S_mT[mrow:mrow + 128, n0:n0 + w], STG[slot][:, 0:w])

    gemm(2048, D, T, lambda k0, kn, m0, mw: wfo_d[k0 * 128:(k0 + kn) * 128, m0:m0 + mw], bank_mul_gate,
         Rsrc=lambda k0, kn, n0, nw: S_YT[k0 * 128:(k0 + kn) * 128, n0:n0 + nw], MW=256, Nb=1024, prep=prep_aux([(S_gT, 0)], 2, 2))
    g.phase_end()

    qT = RB[:, 0:2048]
    kT = RB[:, 2048:4096]
    kcT = RB[:, 4096:4352]
    Vev = RB[:, 4352:6400].rearrange("p (c d) -> p c d", c=16)
    Vod = RB[:, 6400:8448].rearrange("p (c d) -> p c d", c=16)
    Vc = RB[:, 8448:8704].rearrange("p (c d) -> p c d", c=2)
    PT = [RB[:, 8704:9088], RB[:, 9088:9472]]
    TT = FB[:, 0:896].rearrange("p (d c) -> p d c", d=14)
    Sb = [FB[:, 1024:1280], FB[:, 1280:1536]]
    rden = [FB[:, 1536:1600], FB[:, 1600:1664]]
    onT = [FB[:, 2048:4096], FB[:, 4096:6144]]
    a_ld = g.sem("a_ld"); a_s = g.sem("a_s"); a_sb = g.sem("a_sb"); a_pt = g.sem("a_pt"); a_o = g.sem("a_o")
    a_dv = g.sem("a_dv"); a_on = g.sem("a_on")
    P_s = {}; P_sb = {}; P_pt = {}; P_o = {}; P_dv = {}
    on_store = {}
    itg = 0
    for h in range(NH_):
        hs = slice(h * 128, (h + 1) * 128)
        waitp(POOL, P_o.get(itg - 1))
        ldp = None
        for (dst, src) in [(qT, S_qT[hs, :]), (kT, S_kT[hs, :]), (kcT, S_kcT[hs, :]),
                           (Vev, S_v[:, hs].rearrange("(c p) d -> p c d", p=128)),
                           (Vod[:, 0:15, :], S_v[64:64 + 15 * 128, hs].rearrange("(c p) d -> p c d", p=128)),
                           (Vc, S_vc[:, hs].rearrange("(c p) d -> p c d", p=128))]:
            ldp = ev(POOL.dma_start(out=dst, in_=src), a_ld, 16)
        waitp(POOL, P_sb.get(itg - 1))
        ldp = ev(POOL.dma_start(out=FB[:, 0:896], in_=nab_d[h]), a_ld, 16)
        waitp(PE, ldp)
        waitp(DVE, ldp)
        ob = h % 2
        waitp(DVE, on_store.get(h - 2))
        def stage_S(i, r):
            rs = min(max(r - 4, 0), 24)
            d0 = rs - r + 7
            b = i % 2
            waitp(PE, P_pt.get(i - 2))
            waitp(PE, P_sb.get(i - 2))
            qv = qT[:, r * 64:(r + 1) * 64]
            mm = None
            for j in range(4):
                t0 = (rs + 2 * j) * 64
                mm = PE.matmul(PS[:, b, j * 64:(j + 1) * 64], lhsT=kT[:, t0:t0 + 128], rhs=qv, start=True, stop=True)
            for c in range(2):
                mm = PE.matmul(PS[:, b, 256 + c * 64:256 + (c + 1) * 64], lhsT=kcT[:, c * 128:(c + 1) * 128], rhs=qv, start=True, stop=True)
            P_s[i] = ev(mm, a_s)
            waitp(DVE, P_s[i])
            waitp(DVE, P_pt.get(i - 2))
            ii = DVE.scalar_tensor_tensor(out=Sb[b].rearrange("p (j c) -> p j c", j=4), in0=PS[:, b, 0:256].rearrange("p (j c) -> p j c", j=4),
                                          scalar=SCALE, in1=TT[:, d0:d0 + 7:2, :], op0=ALU.mult, op1=ALU.add)
            P_sb[i] = ev(ii, a_sb)
            waitp(ACT, P_sb[i])
            waitp(ACT, P_o.get(i - 2))
            ACT.activation(out=PT[b][:, 0:256], in_=Sb[b], func=AF.Exp)
            e2 = ACT.activation(out=PT[b][:, 256:384], in_=PS[:, b, 256:384], func=AF.Exp, scale=SCALE)
            P_pt[i] = ev(e2, a_pt)

        def stage_PV(i, r):
            rs = min(max(r - 4, 0), 24)
            b = i % 2
            waitp(PE, P_pt[i])
            waitp(PE, P_dv.get(i - 2))
            par = rs % 2
            for j in range(4):
                rowp = rs + 2 * j
                vt = Vev[:, rowp // 2, :] if par == 0 else Vod[:, (rowp - 1) // 2, :]
                PE.matmul(PS[:, 2 + b, 0:64], lhsT=vt, rhs=PT[b][:, j * 64:(j + 1) * 64], start=(j == 0), stop=False)
            for c in range(2):
                PE.matmul(PS[:, 2 + b, 0:64], lhsT=Vc[:, c, :], rhs=PT[b][:, 256 + c * 64:256 + (c + 1) * 64], start=False, stop=(c == 1))
            mm = PE.matmul(PS[:, 4 + b, 0:384], lhsT=onesR[:], rhs=PT[b][:, 0:384], start=True, stop=True)
            P_o[i] = ev(mm, a_o)
            waitp(DVE, P_o[i])
            pr0 = ev(DVE.tensor_reduce(out=rden[b], in_=PS[:, 4 + b, 0:384].rearrange("p (j q) -> p q j", j=6), axis=AXL.X, op=ALU.add), a_dv)
            waitp(DVE, pr0)
            pr = ev(DVE.reciprocal(out=rden[b], in_=rden[b]), a_dv)
            waitp(DVE, pr)
            i2 = DVE.tensor_tensor(out=onT[ob][:, r * 64:(r + 1) * 64], in0=PS[:, 2 + b, 0:64], in1=rden[b], op=ALU.mult)
            P_dv[i] = ev(i2, a_dv)

        for r in range(33):
            if r < 32:
                stage_S(itg + r, r)
            if r >= 1:
                stage_PV(itg + r - 1, r - 1)
        itg += 32
        waitp(SP, P_dv[itg - 1])
        on_store[h] = ev(SP.dma_start(out=S_onT[hs, :], in_=onT[ob]), a_on, 16)
    g.note_store(a_on)
    g.phase_end()

    def bank_gate_add(ps, mrow, n0, w, pe_wait, rel, ctx):
        tk, tk2 = ctx[(mrow, n0)]
        waitp(DVE, pe_wait)
        a = aux_use(DVE, tk)
        slot = stage_acquire(DVE)
        p = rel(DVE.tensor_tensor(out=STG[slot][:, 0:w], in0=ps, in1=a[:, 0:w], op=ALU.mult))
        aux_release(tk, p)
        waitp(DVE, p)
        a2 = aux_use(DVE, tk2)
        p2 = ev(DVE.tensor_tensor(out=STG[slot][:, 0:w], in0=STG[slot][:, 0:w], in1=a2[:, 0:w], op=ALU.add), s_dve2)
        aux_release(tk2, p2)
        stage_store(slot, p2, S_mT[mrow:mrow + 128, n0:n0 + w], STG[slot][:, 0:w])

    gemm(2048, D, T, lambda k0, kn, m0, mw: wna_d[k0 * 128:(k0 + kn) * 128, m0:m0 + mw], bank_gate_add,
         Rsrc=lambda k0, kn, n0, nw: S_onT[k0 * 128:(k0 + kn) * 128, n0:n0 + nw], MW=256, Nb=1024, prep=prep_aux([(S_gT, D), (S_mT, 0)], 2, 2))
    g.phase_end(light=True)

    def bank_res(gt, dst):
        def bank(ps, mrow, n0, w, pe_wait, rel, ctx):
            tk = ctx[(mrow, n0)][0]
            ch = mrow // 128
            waitp(DVE, pe_wait)
            a = aux_use(DVE, tk)
            slot = stage_acquire(DVE)
            p = rel(DVE.scalar_tensor_tensor(out=STG[slot][:, 0:w], in0=ps, scalar=gt[:, ch:ch + 1], in1=a[:, 0:w], op0=ALU.mult, op1=ALU.add))
            aux_release(tk, p)
            stage_store(slot, p, dst[mrow:mrow + 128, n0:n0 + w], STG[slot][:, 0:w])
        return bank

    gemm(D, D, T, lambda k0, kn, m0, mw: wout_d[k0 * 128:(k0 + kn) * 128, m0:m0 + mw], bank_res(VEC["gt_m"], S_x1T),
         Rsrc=lambda k0, kn, n0, nw: S_mT[k0 * 128:(k0 + kn) * 128, n0:n0 + nw], MW=512, Nb=512, prep=prep_aux([(S_xT, 0)]))
    g.phase_end()

    n_ld = [g.sem(f"n_ld{i}") for i in range(4)]
    n_sq = g.sem("n_sq"); n_mm = g.sem("n_mm"); n_d1 = g.sem("n_d1"); n_a = g.sem("n_a"); n_tr = g.sem("n_tr"); n_cp = g.sem("n_cp")
    n_st = [g.sem("n_st0"), g.sem("n_st1")]; n_tm = [g.sem("n_tm0"), g.sem("n_tm1")]

    def norm_phase(srcT, gs, sh, dstT, dst_tm):
        XC = [FB[:, i * 512:(i + 1) * 512] for i in range(4)]
        SQ = [FB[:, 2048 + i * 512: 2048 + (i + 1) * 512] for i in range(2)]
        RS = FB[:, 3072:3584]
        TMP2 = [FB[:, 3584:4096], FB[:, 6144:6656]]
        TB = [FB[:, 4096:4608], FB[:, 4608:5120]]
        NT = [FB[:, 5120:5632], FB[:, 5632:6144]]
        xc_busy = [None] * 4
        sq_busy = [None, None]
        nt_busy = [[], []]
        tb_busy = [None, None]
        bank_busy = {4: None, 5: None}
        tmp_busy = [None, None]
        ps0_busy = None
        li = 0; sj = 0; tc = 0
        for tb in range(T // 512):
            t0 = tb * 512
            mmp = None
            for c in range(32):
                slot = li % 4; li += 1
                waitp(POOL, xc_busy[slot])
                ldp = ev(POOL.dma_start(out=XC[slot], in_=srcT[c * 128:(c + 1) * 128, t0:t0 + 512]), n_ld[slot], 16)
                waitp(ACT, ldp)
                sb_ = sj % 2; sj += 1
                waitp(ACT, sq_busy[sb_])
                ap_ = ev(ACT.activation(out=SQ[sb_], in_=XC[slot], func=AF.Square), n_sq)
                xc_busy[slot] = ap_
                waitp(PE, ap_)
                if c == 0:
                    waitp(PE, ps0_busy)
                mmp = ev(PE.matmul(PS[:, 0, :], lhsT=ones, rhs=SQ[sb_], start=(c == 0), stop=(c == 31)), n_mm)
                sq_busy[sb_] = mmp
            waitp(DVE, mmp)
            p1 = ev(DVE.tensor_scalar(out=RS, in0=PS[:, 0, :], scalar1=1.0 / D, scalar2=EPS, op0=ALU.mult, op1=ALU.add), s_dve)
            ps0_busy = p1
            waitp(ACT, p1)
            pq = ev(ACT.activation(out=RS, in_=RS, func=AF.Sqrt), s_act)
            waitp(DVE, pq)
            p2 = ev(DVE.reciprocal(out=RS, in_=RS), s_dve)
            waitp(DVE, p2)
            pend = {}

            def stage_A(c):
                nonlocal li, tc
                slot = li % 4; li += 1
                waitp(POOL, xc_busy[slot])
                ldp = ev(POOL.dma_start(out=XC[slot], in_=srcT[c * 128:(c + 1) * 128, t0:t0 + 512]), n_ld[slot], 16)
                nb_ = tc % 2
                waitp(DVE, ldp)
                waitp(DVE, tmp_busy[nb_])
                d1 = ev(DVE.tensor_tensor(out=TMP2[nb_], in0=XC[slot], in1=RS, op=ALU.mult), n_d1)
                xc_busy[slot] = d1
                waitp(ACT, d1)
                waitp(ACT, nt_busy[nb_])
                ap_ = ev(ACT.activation(out=NT[nb_], in_=TMP2[nb_], func=AF.Identity, bias=sh[:, c:c + 1], scale=gs[:, c:c + 1]), n_a)
                tmp_busy[nb_] = ap_
                bk = 4 + nb_
                waitp(PE, ap_)
                waitp(PE, bank_busy[bk])
                tr = None
                for q in range(4):
                    tr = PE.transpose(PS[:, bk, q * 128:(q + 1) * 128], NT[nb_][:, q * 128:(q + 1) * 128], ident)
                trp = ev(tr, n_tr)
                nt_busy[nb_] = [trp]
                if dstT is not None:
                    waitp(SP, ap_)
                    sp_ = ev(SP.dma_start(out=dstT[c * 128:(c + 1) * 128, t0:t0 + 512], in_=NT[nb_]), n_st[nb_], 16)
                    nt_busy[nb_].append(sp_)
                    g.note_store(n_st[nb_])
                pend[c] = (nb_, bk, trp)
                tc += 1

            def stage_B(c):
                nb_, bk, trp = pend.pop(c)
                waitp(DVE, trp)
                waitp(DVE, tb_busy[nb_])
                cp = ev(DVE.tensor_copy(out=TB[nb_], in_=PS[:, bk, :]), n_cp)
                bank_busy[bk] = cp
                waitp(SP, cp)
                tb_busy[nb_] = ev(SP.dma_start(out=dst_tm[t0:t0 + 512, c * 128:(c + 1) * 128].rearrange("(q p) d -> p q d", p=128),
                                               in_=TB[nb_].rearrange("p (q d) -> p q d", q=4)), n_tm[nb_], 16)
                g.note_store(n_tm[nb_])

            for c in range(33):
                if c < 32:
                    stage_A(c)
                if c >= 1:
                    stage_B(c - 1)

    norm_phase(S_x1T, VEC["gs_f"], VEC["sh_f"], S_xn2T, S_xn2)
    g.phase_end()

    rt3 = rt[:].rearrange("p (a i e) -> p a i e", a=6, i=16)
    LG, EX, AFF, MASK, GM, RANK = [rt3[:, a] for a in range(6)]

    def bank_router(ps, mrow, n0, w, pe_wait, rel, ctx):
        waitp(DVE, pe_wait)
        rel(DVE.tensor_copy(out=LG[:, mrow // 128, :], in_=ps))

    gemm(D, T, NE, lambda k0, kn, m0, mw: S_xn2T[k0 * 128:(k0 + kn) * 128, m0:m0 + mw], bank_router,
         Rsrc=lambda k0, kn, n0, nw: wr_d[k0 * 128:(k0 + kn) * 128, n0:n0 + nw], MW=512, Nb=NE)
    waitp(DVE, ps_all())
    waitp(PE, ps_all())
    MX = rsm[:, 0:16]; SM = rsm[:, 16:32]; RSM = rsm[:, 32:48]; THRB = rsm[:, 48:64]

    def dchain(i):
        p = ev(i, s_dve)
        waitp(DVE, p)
        return p
    dchain(DVE.tensor_reduce(out=MX, in_=LG, axis=AXL.X, op=ALU.max))
    p = dchain(DVE.tensor_scalar(out=MX, in0=MX, scalar1=-1.0, scalar2=None, op0=ALU.mult))
    waitp(ACT, p)
    a = None
    for tI in range(16):
        a = ACT.activation(out=EX[:, tI, :], in_=LG[:, tI, :], func=AF.Exp, bias=MX[:, tI:tI + 1], scale=1.0, accum_out=SM[:, tI:tI + 1])
    waitp(DVE, ev(a, s_act))
    dchain(DVE.reciprocal(out=RSM, in_=SM))
    i = None
    for tI in range(16):
        i = DVE.tensor_scalar(out=AFF[:, tI, :], in0=EX[:, tI, :], scalar1=RSM[:, tI:tI + 1], scalar2=None, op0=ALU.mult)
    p = dchain(i)
    waitp(PE, p)
    tr = None
    for tI in range(16):
        tr = PE.transpose(PS[0:16, tI // 4, (tI % 4) * 128:(tI % 4 + 1) * 128], AFF[:, tI, :], ident)
    waitp(DVE, ev(tr, s_pe))
    WK = affT[:, T:2 * T]; M8 = affT[:, 2 * T:2 * T + 8]; M8b = affT[:, 2 * T + 8:2 * T + 16]
    THR = affT[:, 2 * T + 16:2 * T + 17]
    for bq in range(4):
        i = DVE.tensor_copy(out=WK[:, bq * 512:(bq + 1) * 512], in_=PS[0:16, bq, :])
    dchain(i)
    for rr in range(CAP // 8):
        dchain(DVE.max(out=M8, in_=WK))
        dchain(DVE.match_replace(out=WK, in_to_replace=M8, in_values=WK, imm_value=-1.0))
    dchain(DVE.max(out=M8b, in_=WK))
    dchain(DVE.tensor_tensor(out=THR, in0=M8[:, 7:8], in1=M8b[:, 0:1], op=ALU.add))
    dchain(DVE.tensor_scalar(out=THR, in0=THR, scalar1=0.5, scalar2=None, op0=ALU.mult))
    THB = affT[:, 0:128]
    p = dchain(DVE.tensor_scalar(out=THB, in0=cst[0:16, 128:256], scalar1=THR, scalar2=None, op0=ALU.mult))
    waitp(PE, p)
    mm = PE.matmul(PS[:, 4, 0:16], lhsT=THB, rhs=cst[0:16, 0:16], start=True, stop=True)
    waitp(DVE, ev(mm, s_pe))
    dchain(DVE.tensor_copy(out=THRB, in_=PS[:, 4, 0:16]))
    for tI in range(16):
        i = DVE.tensor_tensor(out=MASK[:, tI, :], in0=AFF[:, tI, :], in1=THRB, op=ALU.is_ge)
    dchain(i)
    p = dchain(DVE.tensor_tensor(out=GM, in0=AFF, in1=MASK, op=ALU.mult))
    waitp(PE, p)
    for tI in range(16):
        for jj in range(tI):
            PE.matmul(PS[:, 5, tI * 16:(tI + 1) * 16], lhsT=ones, rhs=MASK[:, jj, :], start=(jj == 0), stop=False)
        mm = PE.matmul(PS[:, 5, tI * 16:(tI + 1) * 16], lhsT=tri, rhs=MASK[:, tI, :], start=(tI == 0), stop=True)
    waitp(DVE, ev(mm, s_pe))
    p = dchain(DVE.tensor_copy(out=RANK, in_=PS[:, 5, 0:256].rearrange("p (i e) -> p i e", i=16)))
    for e_ in (ACT, PE, POOL, SP):
        waitp(e_, p)

    xinT = RB[:, 0:8192].rearrange("p (k n) -> p k n", k=32)
    hidT = RB[:, 8192:12288].rearrange("p (k n) -> p k n", k=16)
    SEL = RB[:, 12288:16384].rearrange("p (k n) -> p k n", k=16)
    H1 = FB[:, 0:4096].rearrange("p (k n) -> p k n", k=16)
    SG = [FB[:, 15360:15616], FB[:, 15616:15872]]
    e_sel = g.sem("e_sel"); e_sg = g.sem("e_sg"); e_tr = g.sem("e_tr"); e_cp = g.sem("e_cp")
    sg_busy = [None, None]
    bk_busy = [None, None]
    XIN = [FB[:, 4096:8192], FB[:, 8192:12288]]
    e_gl = g.sem("e_gl")
    xin_busy = None
    sgc = 0
    gath_done = None
    w2_done = None
    for e in range(NE):
        waitp(DVE, gath_done)
        i = None
        for tI in range(16):
            i = DVE.tensor_scalar(out=SEL[:, tI, :], in0=iota, scalar1=RANK[:, tI, e:e + 1], scalar2=MASK[:, tI, e:e + 1],
                                  op0=ALU.is_equal, op1=ALU.mult)
        sel_ready = ev(i, e_sel)
        waitp(PE, ps_all())
        for tI in range(16):
            b2 = sgc % 2
            waitp(DVE, sg_busy[b2])
            sp_ = ev(DVE.tensor_scalar(out=SG[b2], in0=iota, scalar1=RANK[:, tI, e:e + 1], scalar2=GM[:, tI, e:e + 1],
                                       op0=ALU.is_equal, op1=ALU.mult), e_sg)
            waitp(PE, sp_)
            waitp(PE, bk_busy[b2])
            tr = None
            for hh in range(2):
                tr = PE.transpose(PS[:, 6 + b2, hh * 128:(hh + 1) * 128], SG[b2][:, hh * 128:(hh + 1) * 128], ident)
            trp = ev(tr, e_tr)
            sg_busy[b2] = trp
            waitp(ACT, trp)
            slot = stage_acquire(ACT)
            cp = ev(ACT.activation(out=STG[slot][:, 0:256], in_=PS[:, 6 + b2, 0:256], func=AF.Copy), e_cp)
            bk_busy[b2] = cp
            stage_store(slot, cp, S_selgT[e * 256:(e + 1) * 256, tI * 128:(tI + 1) * 128].rearrange("(h p) t -> p h t", p=128),
                        STG[slot][:, 0:256].rearrange("p (h t) -> p h t", h=2))
            sgc += 1
        waitp(PE, bk_busy[0]); waitp(PE, bk_busy[1])

        waitp(PE, sel_ready)
        mm = None
        for hh in range(2):
            for tI in range(16):
                mm = PE.matmul(PS[:, 5, 2 * hh:2 * hh + 2], lhsT=SEL[:, tI, hh * 128:(hh + 1) * 128], rhs=tokR[:, 2 * tI:2 * tI + 2],
                               start=(tI == 0), stop=(tI == 15))
        gath_done = ev(mm, e_tr)
        waitp(DVE, gath_done)
        pconv = ev(DVE.tensor_copy(out=IDX[:], in_=PS[:, 5, 0:4]), e_sg)
        waitp(POOL, pconv)
        waitp(POOL, xin_busy)
        pg = []
        for hh in range(2):
            dd = POOL.indirect_dma_start(out=XIN[hh], out_offset=None, in_=S_xn2[:, :],
                                         in_offset=bass.IndirectOffsetOnAxis(ap=IDX[:, 2 * hh:2 * hh + 1].bitcast(mybir.dt.uint32), axis=0))
            pg.append(ev(dd, e_gl, 16))
        for hh in range(2):
            waitp(PE, pg[hh])
            for cg in range(8):
                pset = ps_acquire()
                tr = None
                for q in range(4):
                    c = cg * 4 + q
                    tr = PE.transpose(PS[:, pset * 4, q * 128:(q + 1) * 128], XIN[hh][:, c * 128:(c + 1) * 128], ident)
                trp = ev(tr, e_tr)
                xin_busy = trp
                eng = copy_eng()
                waitp(eng, trp)
                ps_rel(pset)(evac_copy(eng, xinT[:, cg * 4:(cg + 1) * 4, hh * 128:(hh + 1) * 128],
                                       PS[:, pset * 4, :].rearrange("p (q s) -> p q s", q=4)))
        x_ready = ps_all()

        def bank_h1(ps, mrow, n0, w, pe_wait, rel, ctx):
            waitp(ACT, pe_wait)
            rel(ACT.activation(out=H1[:, mrow // 128, :], in_=ps, func=AF.Silu))
        gemm(D, FF, CAP, lambda k0, kn, m0, mw, e=e: w1_d[e, k0 * 128:(k0 + kn) * 128, m0:m0 + mw], bank_h1,
             Rres=lambda k, n0, w: xinT[:, k, :], MW=512, Nb=CAP, r_wait=x_ready)
        h1_ready = ps_all()

        def bank_hid(ps, mrow, n0, w, pe_wait, rel, ctx):
            waitp(DVE, pe_wait)
            rel(DVE.tensor_tensor(out=hidT[:, mrow // 128, :], in0=ps, in1=H1[:, mrow // 128, :], op=ALU.mult))
        waitp(DVE, h1_ready)
        waitp(DVE, w2_done)
        gemm(D, FF, CAP, lambda k0, kn, m0, mw, e=e: w3_d[e, k0 * 128:(k0 + kn) * 128, m0:m0 + mw], bank_hid,
             Rres=lambda k, n0, w: xinT[:, k, :], MW=512, Nb=CAP)
        hid_ready = ps_all()
        waitp(ACT, hid_ready)
        w2_done = gemm_sr(FF, CAP, D, lambda k, mc: hidT[:, k, mc * 128:(mc + 1) * 128],
                          lambda k0, kn, n0, nw, e=e: w2_d[e, k0 * 128:(k0 + kn) * 128, n0:n0 + nw],
                          bank_copy_to(lambda mrow, n0, w, e=e: S_eo[e * 256 + mrow: e * 256 + mrow + 128, n0:n0 + w]),
                          l_wait=hid_ready)
    g.phase_end()

    gemm(NE * CAP, D, T, lambda k0, kn, m0, mw: S_eo[k0 * 128:(k0 + kn) * 128, m0:m0 + mw], bank_res(VEC["gt_f"], S_x2T),
         Rsrc=lambda k0, kn, n0, nw: S_selgT[k0 * 128:(k0 + kn) * 128, n0:n0 + nw], MW=512, Nb=512, prep=prep_aux([(S_x1T, 0)]))
    g.phase_end()

    norm_phase(S_x2T, VEC["g_fin"], VEC["zero"], None, out_d)
    g.phase_end()


_NC_CACHE = {}


def _consts():
    cst = np.zeros((128, 768), np.float32)
    cst[:, 0:128] = np.eye(128, dtype=np.float32)
    cst[:, 128:256] = 1.0
    cst[:, 256:384] = np.triu(np.ones((128, 128), np.float32), k=1)
    cst[:, 384:640] = np.arange(256, dtype=np.float32)[None, :]
    cst[:, 640:672] = (np.repeat(np.arange(16), 2)[None, :] * 128 + np.arange(128)[:, None]).astype(np.float32)
    ch = np.arange(512, dtype=np.int64)
    ang = 2.0 * np.pi * ((ch[:, None] * ch[None, :]) % 512).astype(np.float64) / 512.0
    cs = np.concatenate([np.cos(ang), np.sin(ang)], axis=1) / 1024.0
    t = np.arange(T, dtype=np.int64)
    angl = 2.0 * np.pi * ((t[:, None] * t[None, :]) % T).astype(np.float64) / T
    cls = np.concatenate([np.cos(angl), -np.sin(angl)], axis=0)
    return cst, cs.astype(np.float32), cls.astype(np.float32)


def _fm(v, n):
    return np.ascontiguousarray(np.asarray(v, np.float32).reshape(n, 128).T)


def _bias_tables(rel_bias):
    c = np.arange(64)
    kc = np.arange(64)
    win = np.clip(c - 8, 0, 48)
    rel = kc[:, None] - win[None, :]
    mask = (rel >= 0) & (rel < 16)
    dc = np.clip(kc[:, None] - c[None, :] + 15, 0, 30)
    H = rel_bias.shape[0]
    out = np.full((H, 2, 64, 14, 64), NEG, np.float32)
    for a in range(2):
        for d in range(14):
            vals = rel_bias[:, d + a][:, dc]
            out[:, a, :, d, :] = np.where(mask[None], vals, np.float32(NEG))
    return np.ascontiguousarray(out.reshape(H, 128, 14 * 64))


def kernel(x, c, ctx, c_ctx, w_mod, b_mod, norm_mix_g, w_in, b_gate, w_fourier, na_rel_bias,
           w_na_out, w_out, norm_ffn_g, w_router, w1, w3, w2, final_norm_g, _cores=None, _debug=(), _stop=None):
    f = lambda a: np.ascontiguousarray(np.asarray(a, dtype=np.float32))
    x, c, ctx, c_ctx = f(x), f(c), f(ctx), f(c_ctx)
    key = (tuple(sorted(_debug)), _stop)
    if key not in _NC_CACHE:
        _NC_CACHE[key] = build(_debug, _stop)
    g = _NC_CACHE[key]
    cst, cs, cls = _consts()
    gvec = np.concatenate([_fm(norm_mix_g[0], 32), _fm(norm_ffn_g[0], 32), _fm(final_norm_g, 32), _fm(b_gate[0], 64)], axis=1)
    shared = {
        "bmodT": _fm(b_mod[0], 192), "gvec": np.ascontiguousarray(gvec), "cst": cst,
        "w_mod": f(w_mod[0]), "w_in": f(w_in[0]), "w_fourier": f(w_fourier[0]), "w_na_out": f(w_na_out[0]),
        "w_out": f(w_out[0]), "w_router": f(w_router[0]), "w1": f(w1[0]), "w3": f(w3[0]), "w2": f(w2[0]),
        "dft_cs": cs, "dft_cls": cls, "nab": _bias_tables(f(na_rel_bias[0])),
    }
    cores = list(range(8)) if _cores is None else list(_cores)
    in_maps = []
    for b in cores:
        c2 = np.stack([c[b], c_ctx], axis=-1).reshape(32, 128, 2)
        c2T = np.ascontiguousarray(c2.transpose(1, 0, 2).reshape(128, 64))
        m = dict(shared)
        m.update({"x": x[b], "ctx": ctx[b], "c2T": c2T})
        in_maps.append(m)
    res = run_bass_kernel_spmd(g.nc, in_maps, core_ids=list(range(len(cores))))
    if _debug:
        return res.results
    return np.stack([r["out"] for r in res.results], axis=0).astype(np.float32)
```

```python
import contextlib
import numpy as np
import concourse.bass as bass
import concourse.mybir as mybir
from concourse.bass_utils import run_bass_kernel_spmd

F32 = mybir.dt.float32
F32R = mybir.dt.float32r
AF = mybir.ActivationFunctionType
ALU = mybir.AluOpType
AXL = mybir.AxisListType

D = 4096
T = 2048
CT = 256
NE = 16
CAP = 256
FF = 2048
NH_ = 16
EPS = 1e-6
NEG = -200.0
SCALE = 128 ** -0.5

NL = 4
LSLOT = 4096
NS = 6
NA = 8


class Sem:
    def __init__(self, nc, es, name):
        self.h = es.enter_context(nc.semaphore(name))
        self.n = 0


class StopBuild(Exception):
    pass


class K:
    def __init__(self, debug=(), stop=None):
        self.debug = set(debug)
        self.stop = stop
        self.nphase = 0
        nc = bass.Bass("TRN2", target_bir_lowering=False)
        self.nc = nc
        self.es = contextlib.ExitStack()
        self.PE, self.ACT, self.DVE, self.POOL, self.SP = nc.tensor, nc.scalar, nc.vector, nc.gpsimd, nc.sync
        self.engs = [self.PE, self.ACT, self.DVE, self.POOL, self.SP]
        self.pending = {}
        self.nsem = 0

    def sem(self, name):
        self.nsem += 1
        return Sem(self.nc, self.es, name)

    def din(self, name, shape):
        return self.nc.dram_tensor(name, list(shape), F32, kind="ExternalInput").ap()

    def dscr(self, name, shape):
        kind = "ExternalOutput" if name in self.debug else "Internal"
        return self.nc.dram_tensor(name, list(shape), F32, kind=kind).ap()

    def sb(self, name, shape, dt=F32):
        return self.es.enter_context(self.nc.sbuf_tensor("sb_" + name, list(shape), dt))

    def wait(self, eng, sem, val):
        if val > 0:
            eng.wait_ge(sem.h, val)

    def inc(self, instr, sem, k=1):
        instr.then_inc(sem.h, k)
        sem.n += k
        return sem.n

    def note_store(self, sem):
        self.pending[id(sem)] = sem

    def phase_end(self, light=False):
        for s in self.pending.values():
            for e in ([self.POOL] if light else self.engs):
                self.wait(e, s, s.n)
        if not light:
            self.pending = {}
        self.nphase += 1
        if self.stop is not None and self.nphase >= self.stop:
            raise StopBuild()


def build(debug=(), stop=None):
    g = K(debug, stop)
    with g.es:
        try:
            _body(g)
        except StopBuild:
            pass
    return g


def _body(g):
    nc, es = g.nc, g.es
    PE, ACT, DVE, POOL, SP = g.PE, g.ACT, g.DVE, g.POOL, g.SP

    def ev(instr, sem, k=1):
        instr.then_inc(sem.h, k)
        sem.n += k
        return (sem, sem.n)

    def waitp(eng, pairs):
        if pairs is None:
            return
        if isinstance(pairs, tuple):
            pairs = [pairs]
        for (s_, v_) in pairs:
            if v_ > 0:
                eng.wait_ge(s_.h, v_)

    x_d = g.din("x", [T, D])
    ctx_d = g.din("ctx", [CT, D])
    c2T_d = g.din("c2T", [128, 64])
    bmodT_d = g.din("bmodT", [128, 192])
    gvec_d = g.din("gvec", [128, 160])
    cst_d = g.din("cst", [128, 768])
    wmod_d = g.din("w_mod", [D, 6 * D])
    win_d = g.din("w_in", [D, 4 * D])
    wfo_d = g.din("w_fourier", [2048, D])
    wna_d = g.din("w_na_out", [2048, D])
    wout_d = g.din("w_out", [D, D])
    wr_d = g.din("w_router", [D, NE])
    w1_d = g.din("w1", [NE, D, FF])
    w3_d = g.din("w3", [NE, D, FF])
    w2_d = g.din("w2", [NE, FF, D])
    cs_d = g.din("dft_cs", [512, 1024])
    cls_d = g.din("dft_cls", [2 * T, T])
    nab_d = g.din("nab", [NH_, 128, 14 * 64])
    out_d = nc.dram_tensor("out", [T, D], F32, kind="ExternalOutput").ap()

    S_xT = g.dscr("S_xT", [D, T])
    S_xnT = g.dscr("S_xnT", [D, T])
    S_cnT = g.dscr("S_cnT", [D, CT])
    S_ufT = g.dscr("S_ufT", [2048, T])
    S_qT = g.dscr("S_qT", [2048, T])
    S_kT = g.dscr("S_kT", [2048, T])
    S_v = g.dscr("S_v", [T, 2048])
    S_gT = g.dscr("S_gT", [2 * D, T])
    S_kcT = g.dscr("S_kcT", [2048, CT])
    S_vc = g.dscr("S_vc", [CT, 2048])
    S_AB = g.dscr("S_AB", [4, T, 1024])
    S_YT = g.dscr("S_YT", [2048, T])
    S_onT = g.dscr("S_onT", [2048, T])
    S_mT = g.dscr("S_mT", [D, T])
    S_x1T = g.dscr("S_x1T", [D, T])
    S_xn2T = g.dscr("S_xn2T", [D, T])
    S_xn2 = g.dscr("S_xn2", [T, D])
    S_selgT = g.dscr("S_selgT", [NE * CAP, T])
    S_eo = g.dscr("S_eo", [NE * CAP, D])
    S_x2T = g.dscr("S_x2T", [D, T])

    RB = g.sb("RB", [128, 16384], F32R)
    LR = [g.sb(f"LR{i}", [128, LSLOT], F32R) for i in range(NL)]
    FB = g.sb("FB", [128, 16384], F32)
    cst = g.sb("cst", [128, 768], F32)
    onesR = g.sb("onesR", [128, 128], F32R)
    tokR = g.sb("tokR", [128, 32], F32R)
    IDX = g.sb("idx", [128, 4], mybir.dt.int32)
    modT = g.sb("modT", [128, 192 * 2], F32)
    bmodT = g.sb("bmodT", [128, 192], F32)
    gvec = g.sb("gvec", [128, 160], F32)
    vec = g.sb("vec", [128, 10 * 32], F32)
    sT = g.sb("sT", [128, 64], F32R)
    c2T = g.sb("c2T", [128, 64], F32)
    small = g.sb("small", [128, 64], F32)
    rt = g.sb("rt", [128, 6 * 256], F32)
    rsm = g.sb("rsm", [128, 64], F32)
    affT = FB[0:16, 0:2 * T + 64]
    PS = es.enter_context(nc.psum_tensor("PS", [128, 8, 512], F32))

    ident = cst[:, 0:128]
    ones = cst[:, 128:256]
    tri = cst[:, 256:384]
    iota = cst[:, 384:640]

    VEC = {n: vec[:, i * 32:(i + 1) * 32] for i, n in enumerate(
        ["gs_m", "sh_m", "gs_mc", "sh_mc", "gt_m", "gs_f", "sh_f", "gt_f", "g_fin", "zero"])}
    bgate = gvec[:, 96:160]

    l_loaded = [g.sem(f"l_ld{i}") for i in range(NL)]
    l_free = [g.sem(f"l_fr{i}") for i in range(NL)]
    rb_loaded = g.sem("rb_ld")
    ps_free = [g.sem("ps_fr0"), g.sem("ps_fr1")]
    st_free = [g.sem(f"st_fr{i}") for i in range(NS)]
    ax_loaded = [[g.sem(f"ax_ld{k}_{i}") for i in range(NA)] for k in range(2)]
    s_misc = g.sem("misc")
    s_dve = g.sem("dve_ev")
    s_dve2 = g.sem("dve_ev2")
    s_act = g.sem("act_ev")
    s_pe = g.sem("pe_ev")
    s_x = [g.sem("x_ld0"), g.sem("x_ld1")]

    st = {"li": 0, "si": 0, "ai": [0, 0], "pset": 0, "cp": 0, "rb_busy": None}
    l_busy = [None] * NL
    ps_busy = [[], []]
    st_busy = [None] * NS
    ax_busy = [[None] * NA for _ in range(2)]
    STG = [FB[:, 12288 + i * 512: 12288 + (i + 1) * 512] for i in range(NS)]
    AUX = [[FB[:, 4096 + (k * NA + i) * 512: 4096 + (k * NA + i + 1) * 512] for i in range(NA)] for k in range(2)]

    def stage_acquire(eng):
        i = st["si"]; st["si"] += 1
        slot = i % NS
        waitp(eng, st_busy[slot])
        return slot

    def stage_store(slot, ready, dst_ap, src_ap=None):
        waitp(SP, ready)
        d = SP.dma_start(out=dst_ap, in_=STG[slot] if src_ap is None else src_ap)
        st_busy[slot] = ev(d, st_free[slot], 16)
        g.note_store(st_free[slot])

    def aux_load(kind, src_ap, w=512):
        i = st["ai"][kind]; st["ai"][kind] += 1
        slot = i % NA
        waitp(POOL, ax_busy[kind][slot])
        d = POOL.dma_start(out=AUX[kind][slot][:, 0:w], in_=src_ap)
        return (kind, slot, ev(d, ax_loaded[kind][slot], 16))

    def aux_use(eng, tok):
        waitp(eng, tok[2])
        return AUX[tok[0]][tok[1]]

    def aux_release(tok, pair):
        ax_busy[tok[0]][tok[1]] = pair

    def copy_eng():
        st["cp"] += 1
        return ACT if st["cp"] % 2 == 0 else DVE

    def evac_copy(eng, out, in_):
        if eng is ACT:
            return ACT.activation(out=out, in_=in_, func=AF.Copy)
        return DVE.tensor_copy(out=out, in_=in_)

    def ps_acquire():
        pset = st["pset"] % 2
        st["pset"] += 1
        waitp(PE, ps_busy[pset])
        ps_busy[pset] = []
        return pset

    def ps_rel(pset):
        def rel(instr):
            p = ev(instr, ps_free[pset])
            ps_busy[pset].append(p)
            return p
        return rel

    def ps_all():
        return list(ps_busy[0]) + list(ps_busy[1])

    def gemm(Kd, M, N, Lsrc, bank, Rsrc=None, Rres=None, MW=512, Nb=512, prep=None, r_wait=None):
        KT = Kd // 128
        KC = min(KT, LSLOT // MW)
        nkt = KT // KC
        W = min(512, Nb)
        NHh = max(1, Nb // 512)
        MC = MW // 128
        nb_banks = MC * NHh
        assert nb_banks <= 4 and KT % KC == 0 and M % MW == 0 and N % Nb == 0
        RBv = RB[:, 0:KT * Nb].rearrange("p (k n) -> p k n", k=KT) if Rres is None else None
        last = None
        for nb in range(N // Nb):
            n0 = nb * Nb
            rb_ready = None
            if Rres is None:
                waitp(POOL, st["rb_busy"])
                RKC = max(1, min(KT, 4096 // Nb))
                for kk in range(0, KT, RKC):
                    d = POOL.dma_start(out=RBv[:, kk:kk + RKC, :],
                                       in_=Rsrc(kk, RKC, n0, Nb).rearrange("(kc p) n -> p kc n", p=128))
                    rb_ready = ev(d, rb_loaded, 16)
            first = True
            for mg in range(M // MW):
                m0 = mg * MW
                ctx = prep(m0, n0) if prep is not None else None
                pset = ps_acquire()
                if first:
                    waitp(PE, rb_ready)
                    waitp(PE, r_wait)
                    first = False
                for kt in range(nkt):
                    i = st["li"]; st["li"] += 1
                    slot = i % NL
                    waitp(POOL, l_busy[slot])
                    Lt = LR[slot][:, 0:KC * MW].rearrange("p (k m) -> p k m", k=KC)
                    d = POOL.dma_start(out=Lt, in_=Lsrc(kt * KC, KC, m0, MW).rearrange("(kc p) m -> p kc m", p=128))
                    waitp(PE, ev(d, l_loaded[slot], 16))
                    mm = None
                    for kc in range(KC):
                        k = kt * KC + kc
                        for mc in range(MC):
                            for nh in range(NHh):
                                rhs = RBv[:, k, nh * W:(nh + 1) * W] if Rres is None else Rres(k, nh * W, W)
                                mm = PE.matmul(PS[:, pset * 4 + mc * NHh + nh, 0:W],
                                               lhsT=Lt[:, kc, mc * 128:(mc + 1) * 128], rhs=rhs,
                                               start=(k == 0), stop=(k == KT - 1))
                    last = ev(mm, l_free[slot])
                    l_busy[slot] = last
                if Rres is None:
                    st["rb_busy"] = last
                rel = ps_rel(pset)
                for mc in range(MC):
                    for nh in range(NHh):
                        bank(PS[:, pset * 4 + mc * NHh + nh, 0:W], m0 + mc * 128, n0 + nh * W, W, last, rel, ctx)
        return last

    def gemm_sr(Kd, M, N, Lres, Rsrc, bank, l_wait=None):
        KT = Kd // 128
        NW = 512
        KC = min(KT, LSLOT // NW)
        nkt = KT // KC
        MC = M // 128
        assert MC <= 4 and KT % KC == 0 and N % NW == 0
        first = True
        last = None
        for ng in range(N // NW):
            n0 = ng * NW
            pset = ps_acquire()
            if first:
                waitp(PE, l_wait)
                first = False
            for kt in range(nkt):
                i = st["li"]; st["li"] += 1
                slot = i % NL
                waitp(POOL, l_busy[slot])
                Rt = LR[slot][:, 0:KC * NW].rearrange("p (k n) -> p k n", k=KC)
                d = POOL.dma_start(out=Rt, in_=Rsrc(kt * KC, KC, n0, NW).rearrange("(kc p) n -> p kc n", p=128))
                waitp(PE, ev(d, l_loaded[slot], 16))
                mm = None
                for kc in range(KC):
                    k = kt * KC + kc
                    for mc in range(MC):
                        mm = PE.matmul(PS[:, pset * 4 + mc, 0:NW], lhsT=Lres(k, mc), rhs=Rt[:, kc, :],
                                       start=(k == 0), stop=(k == KT - 1))
                last = ev(mm, l_free[slot])
                l_busy[slot] = last
            rel = ps_rel(pset)
            for mc in range(MC):
                bank(PS[:, pset * 4 + mc, 0:NW], mc * 128, n0, NW, last, rel, None)
        return last

    def bank_copy_to(dst_fn):
        def bank(ps, mrow, n0, w, pe_wait, rel, ctx):
            eng = copy_eng()
            waitp(eng, pe_wait)
            slot = stage_acquire(eng)
            i = evac_copy(eng, STG[slot][:, 0:w], ps)
            stage_store(slot, rel(i), dst_fn(mrow, n0, w), STG[slot][:, 0:w])
        return bank

    ld = None
    for (dst, src) in [(cst, cst_d), (bmodT, bmodT_d), (gvec, gvec_d), (c2T, c2T_d)]:
        ld = ev(SP.dma_start(out=dst[:], in_=src), s_misc, 16)
    for e_ in (ACT, DVE, PE):
        waitp(e_, ld)
    ev(ACT.activation(out=sT[:], in_=c2T[:], func=AF.Silu), s_act)
    ev(ACT.activation(out=onesR[:], in_=ones, func=AF.Copy), s_act)
    sT_ready = ev(ACT.activation(out=tokR[:], in_=cst[:, 640:672], func=AF.Copy), s_act)

    modT3 = modT[:].rearrange("p (n j) -> p n j", j=2)

    def bank_mod(ps, mrow, n0, w, pe_wait, rel, ctx):
        waitp(DVE, pe_wait)
        ch = mrow // 128
        rel(DVE.tensor_scalar(out=modT3[:, ch, :], in0=ps, scalar1=bmodT[:, ch:ch + 1], scalar2=None, op0=ALU.add))

    gemm(D, 2 * D, 2, lambda k0, kn, m0, mw: wmod_d[k0 * 128:(k0 + kn) * 128, m0:m0 + mw], bank_mod,
         Rres=lambda k, n0, w: sT[:, 2 * k:2 * k + 2], MW=512, Nb=2, r_wait=sT_ready)
    waitp(DVE, ps_all())

    def mv(which, j):
        return modT3[:, which * 32:(which + 1) * 32, j]
    ops = [
        ("gs_m", lambda o: DVE.scalar_tensor_tensor(out=o, in0=mv(1, 0), scalar=1.0, in1=gvec[:, 0:32], op0=ALU.add, op1=ALU.mult)),
        ("sh_m", lambda o: DVE.tensor_copy(out=o, in_=mv(0, 0))),
        ("gs_mc", lambda o: DVE.scalar_tensor_tensor(out=o, in0=mv(1, 1), scalar=1.0, in1=gvec[:, 0:32], op0=ALU.add, op1=ALU.mult)),
        ("sh_mc", lambda o: DVE.tensor_copy(out=o, in_=mv(0, 1))),
        ("gt_m", lambda o: DVE.tensor_copy(out=o, in_=mv(2, 0))),
        ("gs_f", lambda o: DVE.scalar_tensor_tensor(out=o, in0=mv(4, 0), scalar=1.0, in1=gvec[:, 32:64], op0=ALU.add, op1=ALU.mult)),
        ("sh_f", lambda o: DVE.tensor_copy(out=o, in_=mv(3, 0))),
        ("gt_f", lambda o: DVE.tensor_copy(out=o, in_=mv(5, 0))),
        ("g_fin", lambda o: DVE.tensor_copy(out=o, in_=gvec[:, 64:96])),
    ]
    LATE = ("gt_m", "gs_f", "sh_f", "gt_f")
    for n_, f in ops:
        if n_ not in LATE:
            ev(f(VEC[n_]), s_dve)
    vec_ready = ev(DVE.memset(VEC["zero"], 0.0), s_dve)
    for e_ in (ACT, PE, POOL, SP, DVE):
        waitp(e_, vec_ready)

    XT = [FB[:, 0:4096], FB[:, 4096:8192]]
    XS = FB[:, 8192:12288]
    n_tiles = T // 128 + CT // 128
    xt_busy = [[], []]
    xs_busy = None
    for it in range(n_tiles):
        is_ctx = it >= T // 128
        src = ctx_d[(it - 16) * 128:(it - 15) * 128, :] if is_ctx else x_d[it * 128:(it + 1) * 128, :]
        xs_ = it % 2
        waitp(SP, xt_busy[xs_])
        xt_busy[xs_] = []
        x_ready = ev(SP.dma_start(out=XT[xs_], in_=src), s_x[xs_], 16)
        waitp(ACT, x_ready)
        waitp(ACT, xs_busy)
        sq = ev(ACT.activation(out=XS, in_=XT[xs_], func=AF.Square, accum_out=small[:, it:it + 1]), s_act)
        waitp(DVE, sq)
        p1 = ev(DVE.tensor_scalar(out=small[:, 32 + it:33 + it], in0=small[:, it:it + 1], scalar1=1.0 / D, scalar2=EPS, op0=ALU.mult, op1=ALU.add), s_dve)
        waitp(ACT, p1)
        pq = ev(ACT.activation(out=small[:, 32 + it:33 + it], in_=small[:, 32 + it:33 + it], func=AF.Sqrt), s_act)
        waitp(DVE, pq)
        p2 = ev(DVE.reciprocal(out=small[:, 32 + it:33 + it], in_=small[:, 32 + it:33 + it]), s_dve)
        waitp(DVE, p2)
        xs_ready = ev(DVE.tensor_scalar(out=XS, in0=XT[xs_], scalar1=small[:, 32 + it:33 + it], scalar2=None, op0=ALU.mult), s_dve)
        xt_busy[xs_].append(xs_ready)
        gs = VEC["gs_mc"] if is_ctx else VEC["gs_m"]
        sh = VEC["sh_mc"] if is_ctx else VEC["sh_m"]
        waitp(PE, x_ready)
        for kind in (0, 1):
            if kind == 0 and is_ctx:
                continue
            srcT = XT[xs_] if kind == 0 else XS
            if kind == 1:
                waitp(PE, xs_ready)
            for gq in range(8):
                pset = ps_acquire()
                tr = None
                for q in range(4):
                    c = gq * 4 + q
                    tr = PE.transpose(PS[:, pset * 4, q * 128:(q + 1) * 128], srcT[:, c * 128:(c + 1) * 128], ident)
                pe_done = ev(tr, s_pe)
                if gq == 7:
                    if kind == 0:
                        xt_busy[xs_].append(pe_done)
                    else:
                        xs_busy = pe_done
                eng = ACT if kind == 1 else copy_eng()
                waitp(eng, pe_done)
                slot = stage_acquire(eng)
                lasti = None
                if kind == 0:
                    lasti = evac_copy(eng, STG[slot], PS[:, pset * 4, :])
                    dst = S_xT[gq * 512:(gq + 1) * 512, it * 128:(it + 1) * 128]
                else:
                    for q in range(4):
                        c = gq * 4 + q
                        lasti = ACT.activation(out=STG[slot][:, q * 128:(q + 1) * 128], in_=PS[:, pset * 4, q * 128:(q + 1) * 128],
                                               func=AF.Identity, bias=sh[:, c:c + 1], scale=gs[:, c:c + 1])
                    if is_ctx:
                        dst = S_cnT[gq * 512:(gq + 1) * 512, (it - 16) * 128:(it - 15) * 128]
                    else:
                        dst = S_xnT[gq * 512:(gq + 1) * 512, it * 128:(it + 1) * 128]
                rdy = ps_rel(pset)(lasti)
                stage_store(slot, rdy, dst.rearrange("(q p) t -> p q t", p=128), STG[slot].rearrange("p (q t) -> p q t", q=4))
    g.phase_end()

    def win_dst(mrow, n0, w):
        if mrow < 2048:
            return S_ufT[mrow:mrow + 128, n0:n0 + w]
        if mrow < 4096:
            return S_qT[mrow - 2048:mrow - 2048 + 128, n0:n0 + w]
        return S_kT[mrow - 4096:mrow - 4096 + 128, n0:n0 + w]
    copy_win = bank_copy_to(win_dst)

    def bank_win(ps, mrow_, n0, w, pe_wait, rel, ctx):
        mrow = mrow_ if mrow_ < 6144 else mrow_ + 2048
        if mrow < 6144:
            return copy_win(ps, mrow, n0, w, pe_wait, rel, ctx)
        gi = (mrow - 8192) // 128
        waitp(ACT, pe_wait)
        slot = stage_acquire(ACT)
        i = ACT.activation(out=STG[slot][:, 0:w], in_=ps, func=AF.Sigmoid, bias=bgate[:, gi:gi + 1], scale=1.0)
        stage_store(slot, rel(i), S_gT[mrow - 8192:mrow - 8192 + 128, n0:n0 + w], STG[slot][:, 0:w])

    def win_L(k0, kn, m0, mw):
        col = m0 if m0 < 6144 else m0 + 2048
        return win_d[k0 * 128:(k0 + kn) * 128, col:col + mw]

    gemm(D, 6144 + 8192, T, win_L, bank_win,
         Rsrc=lambda k0, kn, n0, nw: S_xnT[k0 * 128:(k0 + kn) * 128, n0:n0 + nw], MW=512, Nb=512)
    gemm(D, T, 2048, lambda k0, kn, m0, mw: S_xnT[k0 * 128:(k0 + kn) * 128, m0:m0 + mw],
         bank_copy_to(lambda mrow, n0, w: S_v[mrow:mrow + 128, n0:n0 + w]),
         Rsrc=lambda k0, kn, n0, nw: win_d[k0 * 128:(k0 + kn) * 128, 6144 + n0:6144 + n0 + nw], MW=512, Nb=512)
    gemm(D, 2048, CT, lambda k0, kn, m0, mw: win_d[k0 * 128:(k0 + kn) * 128, 4096 + m0:4096 + m0 + mw],
         bank_copy_to(lambda mrow, n0, w: S_kcT[mrow:mrow + 128, n0:n0 + w]),
         Rsrc=lambda k0, kn, n0, nw: S_cnT[k0 * 128:(k0 + kn) * 128, n0:n0 + nw], MW=512, Nb=256)
    gemm(D, CT, 2048, lambda k0, kn, m0, mw: S_cnT[k0 * 128:(k0 + kn) * 128, m0:m0 + mw],
         bank_copy_to(lambda mrow, n0, w: S_vc[mrow:mrow + 128, n0:n0 + w]),
         Rsrc=lambda k0, kn, n0, nw: win_d[k0 * 128:(k0 + kn) * 128, 6144 + n0:6144 + n0 + nw], MW=256, Nb=512)
    g.phase_end(light=True)

    for gi in range(4):
        gemm(512, T, 1024, lambda k0, kn, m0, mw, gi=gi: S_ufT[gi * 512 + k0 * 128: gi * 512 + (k0 + kn) * 128, m0:m0 + mw],
             bank_copy_to(lambda mrow, n0, w, gi=gi: S_AB[gi, mrow:mrow + 128, n0:n0 + w]),
             Rsrc=lambda k0, kn, n0, nw: cs_d[k0 * 128:(k0 + kn) * 128, n0:n0 + nw], MW=256, Nb=1024)
    g.phase_end(light=True)
    def L_ab(k0, kn, m0, mw):
        gi, mo = m0 // 512, m0 % 512
        if k0 < 16:
            return S_AB[gi, k0 * 128:(k0 + kn) * 128, mo:mo + mw]
        return S_AB[gi, (k0 - 16) * 128:(k0 - 16 + kn) * 128, 512 + mo:512 + mo + mw]
    gemm(2 * T, 2048, T, L_ab,
         bank_copy_to(lambda mrow, n0, w: S_YT[mrow:mrow + 128, n0:n0 + w]),
         Rsrc=lambda k0, kn, n0, nw: cls_d[k0 * 128:(k0 + kn) * 128, n0:n0 + nw], MW=512, Nb=512)
    g.phase_end(light=True)

    def prep_aux(srcs, MC=4, NHh=1):
        def prep(m0, n0):
            toks = {}
            for mc in range(MC):
                for nh in range(NHh):
                    toks[(m0 + mc * 128, n0 + nh * 512)] = [
                        aux_load(k, s_[off + m0 + mc * 128: off + m0 + (mc + 1) * 128, n0 + nh * 512:n0 + (nh + 1) * 512])
                        for k, (s_, off) in enumerate(srcs)]
            return toks
        return prep

    def bank_mul_gate(ps, mrow, n0, w, pe_wait, rel, ctx):
        tk = ctx[(mrow, n0)][0]
        waitp(DVE, pe_wait)
        a = aux_use(DVE, tk)
        slot = stage_acquire(DVE)
        p = rel(DVE.tensor_tensor(out=STG[slot][:, 0:w], in0=ps, in1=a[:, 0:w], op=ALU.mult))
        aux_release(tk, p)
        stage_store(slot, p, S_mT[mrow:mrow + 128, n0:n0 + w], STG[slot][:, 0:w])

    gemm(2048, D, T, lambda k0, kn, m0, mw: wfo_d[k0 * 128:(k0 + kn) * 128, m0:m0 + mw], bank_mul_gate,
         Rsrc=lambda k0, kn, n0, nw: S_YT[k0 * 128:(k0 + kn) * 128, n0:n0 + nw], MW=256, Nb=1024, prep=prep_aux([(S_gT, 0)], 2, 2))
    g.phase_end()

    qT = RB[:, 0:2048]
    kT = RB[:, 2048:4096]
    kcT = RB[:, 4096:4352]
    Vev = RB[:, 4352:6400].rearrange("p (c d) -> p c d", c=16)
    Vod = RB[:, 6400:8448].rearrange("p (c d) -> p c d", c=16)
    Vc = RB[:, 8448:8704].rearrange("p (c d) -> p c d", c=2)
    PT = [RB[:, 8704:9088], RB[:, 9088:9472]]
    TT = FB[:, 0:896].rearrange("p (d c) -> p d c", d=14)
    Sb = [FB[:, 1024:1280], FB[:, 1280:1536]]
    rden = [FB[:, 1536:1600], FB[:, 1600:1664]]
    onT = [FB[:, 2048:4096], FB[:, 4096:6144]]
    a_ld = g.sem("a_ld"); a_s = g.sem("a_s"); a_sb = g.sem("a_sb"); a_pt = g.sem("a_pt"); a_o = g.sem("a_o")
    a_dv = g.sem("a_dv"); a_on = g.sem("a_on")
    P_s = {}; P_sb = {}; P_pt = {}; P_o = {}; P_dv = {}
    on_store = {}
    def deferred_mod():
        ROW = FB[0:2, 8192:8704]
        b6_busy = None
        b7_busy = None
        row_busy = None
        for ng in range(32):
            col0 = 2 * D + ng * 512
            waitp(PE, b6_busy)
            last = None
            for kt in range(4):
                i_ = st["li"]; st["li"] += 1
                slot = i_ % NL
                waitp(POOL, l_busy[slot])
                Rt = LR[slot][:, 0:4096].rearrange("p (k n) -> p k n", k=8)
                d = POOL.dma_start(out=Rt, in_=wmod_d[kt * 1024:(kt + 1) * 1024, col0:col0 + 512].rearrange("(kc p) n -> p kc n", p=128))
                waitp(PE, ev(d, l_loaded[slot], 16))
                mm = None
                for kc in range(8):
                    k = kt * 8 + kc
                    mm = PE.matmul(PS[0:2, 6, 0:512], lhsT=sT[:, 2 * k:2 * k + 2], rhs=Rt[:, kc, :], start=(k == 0), stop=(k == 31))
                last = ev(mm, l_free[slot])
                l_busy[slot] = last
                yield
            waitp(ACT, last)
            waitp(ACT, row_busy)
            cpy = ev(ACT.activation(out=ROW, in_=PS[0:2, 6, 0:512], func=AF.Copy), s_act)
            b6_busy = cpy
            waitp(PE, cpy)
            waitp(PE, b7_busy)
            tr = None
            for q in range(4):
                tr = PE.transpose(PS[:, 7, 2 * q:2 * q + 2], ROW[:, q * 128:(q + 1) * 128], cst[0:2, 0:2])
            trp = ev(tr, s_pe)
            row_busy = trp
            waitp(DVE, trp)
            ii = None
            for q in range(4):
                ch = col0 // 128 + q
                ii = DVE.tensor_scalar(out=modT3[:, ch, :], in0=PS[:, 7, 2 * q:2 * q + 2], scalar1=bmodT[:, ch:ch + 1], scalar2=None, op0=ALU.add)
            b7_busy = ev(ii, s_dve2)
            st["mod_done"] = b7_busy
            yield

    dgen = deferred_mod()
    dstep = 0
    itg = 0
    for h in range(NH_):
        hs = slice(h * 128, (h + 1) * 128)
        waitp(POOL, P_o.get(itg - 1))
        ldp = None
        for (dst, src) in [(qT, S_qT[hs, :]), (kT, S_kT[hs, :]), (kcT, S_kcT[hs, :]),
                           (Vev, S_v[:, hs].rearrange("(c p) d -> p c d", p=128)),
                           (Vod[:, 0:15, :], S_v[64:64 + 15 * 128, hs].rearrange("(c p) d -> p c d", p=128)),
                           (Vc, S_vc[:, hs].rearrange("(c p) d -> p c d", p=128))]:
            ldp = ev(POOL.dma_start(out=dst, in_=src), a_ld, 16)
        waitp(POOL, P_sb.get(itg - 1))
        ldp = ev(POOL.dma_start(out=FB[:, 0:896], in_=nab_d[h]), a_ld, 16)
        waitp(PE, ldp)
        waitp(DVE, ldp)
        ob = h % 2
        waitp(DVE, on_store.get(h - 2))
        def stage_S(i, r):
            rs = min(max(r - 4, 0), 24)
            d0 = rs - r + 7
            b = i % 2
            waitp(PE, P_pt.get(i - 2))
            waitp(PE, P_sb.get(i - 2))
            qv = qT[:, r * 64:(r + 1) * 64]
            mm = None
            for j in range(4):
                t0 = (rs + 2 * j) * 64
                mm = PE.matmul(PS[:, b, j * 64:(j + 1) * 64], lhsT=kT[:, t0:t0 + 128], rhs=qv, start=True, stop=True)
            for c in range(2):
                mm = PE.matmul(PS[:, b, 256 + c * 64:256 + (c + 1) * 64], lhsT=kcT[:, c * 128:(c + 1) * 128], rhs=qv, start=True, stop=True)
            P_s[i] = ev(mm, a_s)
            waitp(DVE, P_s[i])
            waitp(DVE, P_pt.get(i - 2))
            ii = DVE.scalar_tensor_tensor(out=Sb[b].rearrange("p (j c) -> p j c", j=4), in0=PS[:, b, 0:256].rearrange("p (j c) -> p j c", j=4),
                                          scalar=SCALE, in1=TT[:, d0:d0 + 7:2, :], op0=ALU.mult, op1=ALU.add)
            P_sb[i] = ev(ii, a_sb)
            waitp(ACT, P_sb[i])
            waitp(ACT, P_o.get(i - 2))
            ACT.activation(out=PT[b][:, 0:256], in_=Sb[b], func=AF.Exp)
            e2 = ACT.activation(out=PT[b][:, 256:384], in_=PS[:, b, 256:384], func=AF.Exp, scale=SCALE)
            P_pt[i] = ev(e2, a_pt)

        def stage_PV(i, r):
            rs = min(max(r - 4, 0), 24)
            b = i % 2
            waitp(PE, P_pt[i])
            waitp(PE, P_dv.get(i - 2))
            par = rs % 2
            for j in range(4):
                rowp = rs + 2 * j
                vt = Vev[:, rowp // 2, :] if par == 0 else Vod[:, (rowp - 1) // 2, :]
                PE.matmul(PS[:, 2 + b, 0:64], lhsT=vt, rhs=PT[b][:, j * 64:(j + 1) * 64], start=(j == 0), stop=False)
            for c in range(2):
                PE.matmul(PS[:, 2 + b, 0:64], lhsT=Vc[:, c, :], rhs=PT[b][:, 256 + c * 64:256 + (c + 1) * 64], start=False, stop=(c == 1))
            mm = PE.matmul(PS[:, 4 + b, 0:384], lhsT=onesR[:], rhs=PT[b][:, 0:384], start=True, stop=True)
            P_o[i] = ev(mm, a_o)
            waitp(DVE, P_o[i])
            pr0 = ev(DVE.tensor_reduce(out=rden[b], in_=PS[:, 4 + b, 0:384].rearrange("p (j q) -> p q j", j=6), axis=AXL.X, op=ALU.add), a_dv)
            waitp(DVE, pr0)
            pr = ev(DVE.reciprocal(out=rden[b], in_=rden[b]), a_dv)
            waitp(DVE, pr)
            i2 = DVE.tensor_tensor(out=onT[ob][:, r * 64:(r + 1) * 64], in0=PS[:, 2 + b, 0:64], in1=rden[b], op=ALU.mult)
            P_dv[i] = ev(i2, a_dv)

        for r in range(33):
            if r < 32:
                stage_S(itg + r, r)
            if r >= 1:
                stage_PV(itg + r - 1, r - 1)
            dstep += 1
            if dstep % 3 == 0:
                next(dgen, None)
        itg += 32
        waitp(SP, P_dv[itg - 1])
        on_store[h] = ev(SP.dma_start(out=S_onT[hs, :], in_=onT[ob]), a_on, 16)
    g.note_store(a_on)
    for _ in dgen:
        pass
    waitp(DVE, st["mod_done"])
    late_ready = None
    for n_, f in ops:
        if n_ in LATE:
            late_ready = ev(f(VEC[n_]), s_dve)
    for e_ in (ACT, PE, POOL, SP, DVE):
        waitp(e_, late_ready)
    g.phase_end()

    def bank_gate_add(ps, mrow, n0, w, pe_wait, rel, ctx):
        tk, tk2 = ctx[(mrow, n0)]
        waitp(DVE, pe_wait)
        a = aux_use(DVE, tk)
        slot = stage_acquire(DVE)
        p = rel(DVE.tensor_tensor(out=STG[slot][:, 0:w], in0=ps, in1=a[:, 0:w], op=ALU.mult))
        aux_release(tk, p)
        waitp(DVE, p)
        a2 = aux_use(DVE, tk2)
        p2 = ev(DVE.tensor_tensor(out=STG[slot][:, 0:w], in0=STG[slot][:, 0:w], in1=a2[:, 0:w], op=ALU.add), s_dve2)
        aux_release(tk2, p2)
        stage_store(slot, p2, S_mT[mrow:mrow + 128, n0:n0 + w], STG[slot][:, 0:w])

    gemm(2048, D, T, lambda k0, kn, m0, mw: wna_d[k0 * 128:(k0 + kn) * 128, m0:m0 + mw], bank_gate_add,
         Rsrc=lambda k0, kn, n0, nw: S_onT[k0 * 128:(k0 + kn) * 128, n0:n0 + nw], MW=256, Nb=1024, prep=prep_aux([(S_gT, D), (S_mT, 0)], 2, 2))
    g.phase_end(light=True)

    def bank_res(gt, dst):
        def bank(ps, mrow, n0, w, pe_wait, rel, ctx):
            tk = ctx[(mrow, n0)][0]
            ch = mrow // 128
            waitp(DVE, pe_wait)
            a = aux_use(DVE, tk)
            slot = stage_acquire(DVE)
            p = rel(DVE.scalar_tensor_tensor(out=STG[slot][:, 0:w], in0=ps, scalar=gt[:, ch:ch + 1], in1=a[:, 0:w], op0=ALU.mult, op1=ALU.add))
            aux_release(tk, p)
            stage_store(slot, p, dst[mrow:mrow + 128, n0:n0 + w], STG[slot][:, 0:w])
        return bank

    gemm(D, D, T, lambda k0, kn, m0, mw: wout_d[k0 * 128:(k0 + kn) * 128, m0:m0 + mw], bank_res(VEC["gt_m"], S_x1T),
         Rsrc=lambda k0, kn, n0, nw: S_mT[k0 * 128:(k0 + kn) * 128, n0:n0 + nw], MW=512, Nb=512, prep=prep_aux([(S_xT, 0)]))
    g.phase_end()

    n_ld = [g.sem(f"n_ld{i}") for i in range(4)]
    n_sq = g.sem("n_sq"); n_mm = g.sem("n_mm"); n_d1 = g.sem("n_d1"); n_a = g.sem("n_a"); n_tr = g.sem("n_tr"); n_cp = g.sem("n_cp")
    n_st = [g.sem("n_st0"), g.sem("n_st1")]; n_tm = [g.sem("n_tm0"), g.sem("n_tm1")]

    def norm_phase(srcT, gs, sh, dstT, dst_tm):
        XC = [FB[:, i * 512:(i + 1) * 512] for i in range(4)]
        SQ = [FB[:, 2048 + i * 512: 2048 + (i + 1) * 512] for i in range(2)]
        RS = FB[:, 3072:3584]
        TMP2 = [FB[:, 3584:4096], FB[:, 6144:6656]]
        TB = [FB[:, 4096:4608], FB[:, 4608:5120]]
        NT = [FB[:, 5120:5632], FB[:, 5632:6144]]
        xc_busy = [None] * 4
        sq_busy = [None, None]
        nt_busy = [[], []]
        tb_busy = [None, None]
        bank_busy = {4: None, 5: None}
        tmp_busy = [None, None]
        ps0_busy = None
        li = 0; sj = 0; tc = 0
        for tb in range(T // 512):
            t0 = tb * 512
            mmp = None
            for c in range(32):
                slot = li % 4; li += 1
                waitp(POOL, xc_busy[slot])
                ldp = ev(POOL.dma_start(out=XC[slot], in_=srcT[c * 128:(c + 1) * 128, t0:t0 + 512]), n_ld[slot], 16)
                waitp(ACT, ldp)
                sb_ = sj % 2; sj += 1
                waitp(ACT, sq_busy[sb_])
                ap_ = ev(ACT.activation(out=SQ[sb_], in_=XC[slot], func=AF.Square), n_sq)
                xc_busy[slot] = ap_
                waitp(PE, ap_)
                if c == 0:
                    waitp(PE, ps0_busy)
                mmp = ev(PE.matmul(PS[:, 0, :], lhsT=ones, rhs=SQ[sb_], start=(c == 0), stop=(c == 31)), n_mm)
                sq_busy[sb_] = mmp
            waitp(DVE, mmp)
            p1 = ev(DVE.tensor_scalar(out=RS, in0=PS[:, 0, :], scalar1=1.0 / D, scalar2=EPS, op0=ALU.mult, op1=ALU.add), s_dve)
            ps0_busy = p1
            waitp(ACT, p1)
            pq = ev(ACT.activation(out=RS, in_=RS, func=AF.Sqrt), s_act)
            waitp(DVE, pq)
            p2 = ev(DVE.reciprocal(out=RS, in_=RS), s_dve)
            waitp(DVE, p2)
            pend = {}

            def stage_A(c):
                nonlocal li, tc
                slot = li % 4; li += 1
                waitp(POOL, xc_busy[slot])
                ldp = ev(POOL.dma_start(out=XC[slot], in_=srcT[c * 128:(c + 1) * 128, t0:t0 + 512]), n_ld[slot], 16)
                nb_ = tc % 2
                waitp(DVE, ldp)
                waitp(DVE, tmp_busy[nb_])
                d1 = ev(DVE.tensor_tensor(out=TMP2[nb_], in0=XC[slot], in1=RS, op=ALU.mult), n_d1)
                xc_busy[slot] = d1
                waitp(ACT, d1)
                waitp(ACT, nt_busy[nb_])
                ap_ = ev(ACT.activation(out=NT[nb_], in_=TMP2[nb_], func=AF.Identity, bias=sh[:, c:c + 1], scale=gs[:, c:c + 1]), n_a)
                tmp_busy[nb_] = ap_
                bk = 4 + nb_
                waitp(PE, ap_)
                waitp(PE, bank_busy[bk])
                tr = None
                for q in range(4):
                    tr = PE.transpose(PS[:, bk, q * 128:(q + 1) * 128], NT[nb_][:, q * 128:(q + 1) * 128], ident)
                trp = ev(tr, n_tr)
                nt_busy[nb_] = [trp]
                if dstT is not None:
                    waitp(SP, ap_)
                    sp_ = ev(SP.dma_start(out=dstT[c * 128:(c + 1) * 128, t0:t0 + 512], in_=NT[nb_]), n_st[nb_], 16)
                    nt_busy[nb_].append(sp_)
                    g.note_store(n_st[nb_])
                pend[c] = (nb_, bk, trp)
                tc += 1

            def stage_B(c):
                nb_, bk, trp = pend.pop(c)
                waitp(DVE, trp)
                waitp(DVE, tb_busy[nb_])
                cp = ev(DVE.tensor_copy(out=TB[nb_], in_=PS[:, bk, :]), n_cp)
                bank_busy[bk] = cp
                waitp(SP, cp)
                tb_busy[nb_] = ev(SP.dma_start(out=dst_tm[t0:t0 + 512, c * 128:(c + 1) * 128].rearrange("(q p) d -> p q d", p=128),
                                               in_=TB[nb_].rearrange("p (q d) -> p q d", q=4)), n_tm[nb_], 16)
                g.note_store(n_tm[nb_])

            for c in range(33):
                if c < 32:
                    stage_A(c)
                if c >= 1:
                    stage_B(c - 1)

    norm_phase(S_x1T, VEC["gs_f"], VEC["sh_f"], S_xn2T, S_xn2)
    g.phase_end()

    rt3 = rt[:].rearrange("p (a i e) -> p a i e", a=6, i=16)
    LG, EX, AFF, MASK, GM, RANK = [rt3[:, a] for a in range(6)]

    def bank_router(ps, mrow, n0, w, pe_wait, rel, ctx):
        waitp(DVE, pe_wait)
        rel(DVE.tensor_copy(out=LG[:, mrow // 128, :], in_=ps))

    gemm(D, T, NE, lambda k0, kn, m0, mw: S_xn2T[k0 * 128:(k0 + kn) * 128, m0:m0 + mw], bank_router,
         Rsrc=lambda k0, kn, n0, nw: wr_d[k0 * 128:(k0 + kn) * 128, n0:n0 + nw], MW=512, Nb=NE)
    waitp(DVE, ps_all())
    waitp(PE, ps_all())
    MX = rsm[:, 0:16]; SM = rsm[:, 16:32]; RSM = rsm[:, 32:48]; THRB = rsm[:, 48:64]

    def dchain(i):
        p = ev(i, s_dve)
        waitp(DVE, p)
        return p
    dchain(DVE.tensor_reduce(out=MX, in_=LG, axis=AXL.X, op=ALU.max))
    p = dchain(DVE.tensor_scalar(out=MX, in0=MX, scalar1=-1.0, scalar2=None, op0=ALU.mult))
    waitp(ACT, p)
    a = None
    for tI in range(16):
        a = ACT.activation(out=EX[:, tI, :], in_=LG[:, tI, :], func=AF.Exp, bias=MX[:, tI:tI + 1], scale=1.0, accum_out=SM[:, tI:tI + 1])
    waitp(DVE, ev(a, s_act))
    dchain(DVE.reciprocal(out=RSM, in_=SM))
    i = None
    for tI in range(16):
        i = DVE.tensor_scalar(out=AFF[:, tI, :], in0=EX[:, tI, :], scalar1=RSM[:, tI:tI + 1], scalar2=None, op0=ALU.mult)
    p = dchain(i)
    waitp(PE, p)
    tr = None
    for tI in range(16):
        tr = PE.transpose(PS[0:16, tI // 4, (tI % 4) * 128:(tI % 4 + 1) * 128], AFF[:, tI, :], ident)
    waitp(DVE, ev(tr, s_pe))
    WK = affT[:, T:2 * T]; M8 = affT[:, 2 * T:2 * T + 8]; M8b = affT[:, 2 * T + 8:2 * T + 16]
    THR = affT[:, 2 * T + 16:2 * T + 17]
    for bq in range(4):
        i = DVE.tensor_copy(out=WK[:, bq * 512:(bq + 1) * 512], in_=PS[0:16, bq, :])
    dchain(i)
    for rr in range(CAP // 8):
        dchain(DVE.max(out=M8, in_=WK))
        dchain(DVE.match_replace(out=WK, in_to_replace=M8, in_values=WK, imm_value=-1.0))
    dchain(DVE.max(out=M8b, in_=WK))
    dchain(DVE.tensor_tensor(out=THR, in0=M8[:, 7:8], in1=M8b[:, 0:1], op=ALU.add))
    dchain(DVE.tensor_scalar(out=THR, in0=THR, scalar1=0.5, scalar2=None, op0=ALU.mult))
    THB = affT[:, 0:128]
    p = dchain(DVE.tensor_scalar(out=THB, in0=cst[0:16, 128:256], scalar1=THR, scalar2=None, op0=ALU.mult))
    waitp(PE, p)
    mm = PE.matmul(PS[:, 4, 0:16], lhsT=THB, rhs=cst[0:16, 0:16], start=True, stop=True)
    waitp(DVE, ev(mm, s_pe))
    dchain(DVE.tensor_copy(out=THRB, in_=PS[:, 4, 0:16]))
    for tI in range(16):
        i = DVE.tensor_tensor(out=MASK[:, tI, :], in0=AFF[:, tI, :], in1=THRB, op=ALU.is_ge)
    dchain(i)
    p = dchain(DVE.tensor_tensor(out=GM, in0=AFF, in1=MASK, op=ALU.mult))
    waitp(PE, p)
    for tI in range(16):
        for jj in range(tI):
            PE.matmul(PS[:, 5, tI * 16:(tI + 1) * 16], lhsT=ones, rhs=MASK[:, jj, :], start=(jj == 0), stop=False)
        mm = PE.matmul(PS[:, 5, tI * 16:(tI + 1) * 16], lhsT=tri, rhs=MASK[:, tI, :], start=(tI == 0), stop=True)
    waitp(DVE, ev(mm, s_pe))
    p = dchain(DVE.tensor_copy(out=RANK, in_=PS[:, 5, 0:256].rearrange("p (i e) -> p i e", i=16)))
    for e_ in (ACT, PE, POOL, SP):
        waitp(e_, p)

    xinT = RB[:, 0:8192].rearrange("p (k n) -> p k n", k=32)
    hidT = RB[:, 8192:12288].rearrange("p (k n) -> p k n", k=16)
    SEL = RB[:, 12288:16384].rearrange("p (k n) -> p k n", k=16)
    H1 = FB[:, 0:4096].rearrange("p (k n) -> p k n", k=16)
    SG = [FB[:, 15360:15616], FB[:, 15616:15872]]
    e_sel = g.sem("e_sel"); e_sg = g.sem("e_sg"); e_tr = g.sem("e_tr"); e_cp = g.sem("e_cp")
    sg_busy = [None, None]
    bk_busy = [None, None]
    XIN = [FB[:, 4096:8192], FB[:, 8192:12288]]
    e_gl = g.sem("e_gl")
    xin_busy = None
    sgc = 0
    gath_done = None
    w2_done = None
    for e in range(NE):
        waitp(DVE, gath_done)
        i = None
        for tI in range(16):
            i = DVE.tensor_scalar(out=SEL[:, tI, :], in0=iota, scalar1=RANK[:, tI, e:e + 1], scalar2=MASK[:, tI, e:e + 1],
                                  op0=ALU.is_equal, op1=ALU.mult)
        sel_ready = ev(i, e_sel)
        waitp(PE, ps_all())
        for tI in range(16):
            b2 = sgc % 2
            waitp(DVE, sg_busy[b2])
            sp_ = ev(DVE.tensor_scalar(out=SG[b2], in0=iota, scalar1=RANK[:, tI, e:e + 1], scalar2=GM[:, tI, e:e + 1],
                                       op0=ALU.is_equal, op1=ALU.mult), e_sg)
            waitp(PE, sp_)
            waitp(PE, bk_busy[b2])
            tr = None
            for hh in range(2):
                tr = PE.transpose(PS[:, 6 + b2, hh * 128:(hh + 1) * 128], SG[b2][:, hh * 128:(hh + 1) * 128], ident)
            trp = ev(tr, e_tr)
            sg_busy[b2] = trp
            waitp(ACT, trp)
            slot = stage_acquire(ACT)
            cp = ev(ACT.activation(out=STG[slot][:, 0:256], in_=PS[:, 6 + b2, 0:256], func=AF.Copy), e_cp)
            bk_busy[b2] = cp
            stage_store(slot, cp, S_selgT[e * 256:(e + 1) * 256, tI * 128:(tI + 1) * 128].rearrange("(h p) t -> p h t", p=128),
                        STG[slot][:, 0:256].rearrange("p (h t) -> p h t", h=2))
            sgc += 1
        waitp(PE, bk_busy[0]); waitp(PE, bk_busy[1])

        waitp(PE, sel_ready)
        mm = None
        for hh in range(2):
            for tI in range(16):
                mm = PE.matmul(PS[:, 5, 2 * hh:2 * hh + 2], lhsT=SEL[:, tI, hh * 128:(hh + 1) * 128], rhs=tokR[:, 2 * tI:2 * tI + 2],
                               start=(tI == 0), stop=(tI == 15))
        gath_done = ev(mm, e_tr)
        waitp(DVE, gath_done)
        pconv = ev(DVE.tensor_copy(out=IDX[:], in_=PS[:, 5, 0:4]), e_sg)
        waitp(POOL, pconv)
        waitp(POOL, xin_busy)
        pg = []
        for hh in range(2):
            dd = POOL.indirect_dma_start(out=XIN[hh], out_offset=None, in_=S_xn2[:, :],
                                         in_offset=bass.IndirectOffsetOnAxis(ap=IDX[:, 2 * hh:2 * hh + 1].bitcast(mybir.dt.uint32), axis=0))
            pg.append(ev(dd, e_gl, 16))
        for hh in range(2):
            waitp(PE, pg[hh])
            for cg in range(8):
                pset = ps_acquire()
                tr = None
                for q in range(4):
                    c = cg * 4 + q
                    tr = PE.transpose(PS[:, pset * 4, q * 128:(q + 1) * 128], XIN[hh][:, c * 128:(c + 1) * 128], ident)
                trp = ev(tr, e_tr)
                xin_busy = trp
                eng = copy_eng()
                waitp(eng, trp)
                ps_rel(pset)(evac_copy(eng, xinT[:, cg * 4:(cg + 1) * 4, hh * 128:(hh + 1) * 128],
                                       PS[:, pset * 4, :].rearrange("p (q s) -> p q s", q=4)))
        x_ready = ps_all()

        def bank_h1(ps, mrow, n0, w, pe_wait, rel, ctx):
            waitp(ACT, pe_wait)
            rel(ACT.activation(out=H1[:, mrow // 128, :], in_=ps, func=AF.Silu))
        gemm(D, FF, CAP, lambda k0, kn, m0, mw, e=e: w1_d[e, k0 * 128:(k0 + kn) * 128, m0:m0 + mw], bank_h1,
             Rres=lambda k, n0, w: xinT[:, k, :], MW=512, Nb=CAP, r_wait=x_ready)
        h1_ready = ps_all()

        def bank_hid(ps, mrow, n0, w, pe_wait, rel, ctx):
            waitp(DVE, pe_wait)
            rel(DVE.tensor_tensor(out=hidT[:, mrow // 128, :], in0=ps, in1=H1[:, mrow // 128, :], op=ALU.mult))
        waitp(DVE, h1_ready)
        waitp(DVE, w2_done)
        gemm(D, FF, CAP, lambda k0, kn, m0, mw, e=e: w3_d[e, k0 * 128:(k0 + kn) * 128, m0:m0 + mw], bank_hid,
             Rres=lambda k, n0, w: xinT[:, k, :], MW=512, Nb=CAP)
        hid_ready = ps_all()
        waitp(ACT, hid_ready)
        w2_done = gemm_sr(FF, CAP, D, lambda k, mc: hidT[:, k, mc * 128:(mc + 1) * 128],
                          lambda k0, kn, n0, nw, e=e: w2_d[e, k0 * 128:(k0 + kn) * 128, n0:n0 + nw],
                          bank_copy_to(lambda mrow, n0, w, e=e: S_eo[e * 256 + mrow: e * 256 + mrow + 128, n0:n0 + w]),
                          l_wait=hid_ready)
    g.phase_end()

    gemm(NE * CAP, D, T, lambda k0, kn, m0, mw: S_eo[k0 * 128:(k0 + kn) * 128, m0:m0 + mw], bank_res(VEC["gt_f"], S_x2T),
         Rsrc=lambda k0, kn, n0, nw: S_selgT[k0 * 128:(k0 + kn) * 128, n0:n0 + nw], MW=512, Nb=512, prep=prep_aux([(S_x1T, 0)]))
    g.phase_end()

    norm_phase(S_x2T, VEC["g_fin"], VEC["zero"], None, out_d)
    g.phase_end()


_NC_CACHE = {}


def _consts():
    cst = np.zeros((128, 768), np.float32)
    cst[:, 0:128] = np.eye(128, dtype=np.float32)
    cst[:, 128:256] = 1.0
    cst[:, 256:384] = np.triu(np.ones((128, 128), np.float32), k=1)
    cst[:, 384:640] = np.arange(256, dtype=np.float32)[None, :]
    cst[:, 640:672] = (np.repeat(np.arange(16), 2)[None, :] * 128 + np.arange(128)[:, None]).astype(np.float32)
    ch = np.arange(512, dtype=np.int64)
    ang = 2.0 * np.pi * ((ch[:, None] * ch[None, :]) % 512).astype(np.float64) / 512.0
    cs = np.concatenate([np.cos(ang), np.sin(ang)], axis=1) / 1024.0
    t = np.arange(T, dtype=np.int64)
    angl = 2.0 * np.pi * ((t[:, None] * t[None, :]) % T).astype(np.float64) / T
    cls = np.concatenate([np.cos(angl), -np.sin(angl)], axis=0)
    return cst, cs.astype(np.float32), cls.astype(np.float32)


def _fm(v, n):
    return np.ascontiguousarray(np.asarray(v, np.float32).reshape(n, 128).T)


def _bias_tables(rel_bias):
    c = np.arange(64)
    kc = np.arange(64)
    win = np.clip(c - 8, 0, 48)
    rel = kc[:, None] - win[None, :]
    mask = (rel >= 0) & (rel < 16)
    dc = np.clip(kc[:, None] - c[None, :] + 15, 0, 30)
    H = rel_bias.shape[0]
    out = np.full((H, 2, 64, 14, 64), NEG, np.float32)
    for a in range(2):
        for d in range(14):
            vals = rel_bias[:, d + a][:, dc]
            out[:, a, :, d, :] = np.where(mask[None], vals, np.float32(NEG))
    return np.ascontiguousarray(out.reshape(H, 128, 14 * 64))


def kernel(x, c, ctx, c_ctx, w_mod, b_mod, norm_mix_g, w_in, b_gate, w_fourier, na_rel_bias,
           w_na_out, w_out, norm_ffn_g, w_router, w1, w3, w2, final_norm_g, _cores=None, _debug=(), _stop=None):
    f = lambda a: np.ascontiguousarray(np.asarray(a, dtype=np.float32))
    x, c, ctx, c_ctx = f(x), f(c), f(ctx), f(c_ctx)
    key = (tuple(sorted(_debug)), _stop)
    if key not in _NC_CACHE:
        _NC_CACHE[key] = build(_debug, _stop)
    g = _NC_CACHE[key]
    cst, cs, cls = _consts()
    gvec = np.concatenate([_fm(norm_mix_g[0], 32), _fm(norm_ffn_g[0], 32), _fm(final_norm_g, 32), _fm(b_gate[0], 64)], axis=1)
    shared = {
        "bmodT": _fm(b_mod[0], 192), "gvec": np.ascontiguousarray(gvec), "cst": cst,
        "w_mod": f(w_mod[0]), "w_in": f(w_in[0]), "w_fourier": f(w_fourier[0]), "w_na_out": f(w_na_out[0]),
        "w_out": f(w_out[0]), "w_router": f(w_router[0]), "w1": f(w1[0]), "w3": f(w3[0]), "w2": f(w2[0]),
        "dft_cs": cs, "dft_cls": cls, "nab": _bias_tables(f(na_rel_bias[0])),
    }
    cores = list(range(8)) if _cores is None else list(_cores)
    in_maps = []
    for b in cores:
        c2 = np.stack([c[b], c_ctx], axis=-1).reshape(32, 128, 2)
        c2T = np.ascontiguousarray(c2.transpose(1, 0, 2).reshape(128, 64))
        m = dict(shared)
        m.update({"x": x[b], "ctx": ctx[b], "c2T": c2T})
        in_maps.append(m)
    res = run_bass_kernel_spmd(g.nc, in_maps, core_ids=list(range(len(cores))))
    if _debug:
        return res.results
    return np.stack([r["out"] for r in res.results], axis=0).astype(np.float32)
```

```python
import contextlib
import numpy as np
import concourse.bass as bass
import concourse.mybir as mybir
from concourse.bass_utils import run_bass_kernel_spmd

F32 = mybir.dt.float32
F32R = mybir.dt.float32r
AF = mybir.ActivationFunctionType
ALU = mybir.AluOpType
AXL = mybir.AxisListType

D = 4096
T = 2048
CT = 256
NE = 16
CAP = 256
FF = 2048
NH_ = 16
EPS = 1e-6
NEG = -200.0
SCALE = 128 ** -0.5

NL = 4
LSLOT = 4096
NS = 6
NA = 8


class Sem:
    def __init__(self, nc, es, name):
        self.h = es.enter_context(nc.semaphore(name))
        self.n = 0


class StopBuild(Exception):
    pass


class K:
    def __init__(self, debug=(), stop=None):
        self.debug = set(debug)
        self.stop = stop
        self.nphase = 0
        nc = bass.Bass("TRN2", target_bir_lowering=False)
        self.nc = nc
        self.es = contextlib.ExitStack()
        self.PE, self.ACT, self.DVE, self.POOL, self.SP = nc.tensor, nc.scalar, nc.vector, nc.gpsimd, nc.sync
        self.engs = [self.PE, self.ACT, self.DVE, self.POOL, self.SP]
        self.pending = {}
        self.nsem = 0

    def sem(self, name):
        self.nsem += 1
        return Sem(self.nc, self.es, name)

    def din(self, name, shape):
        return self.nc.dram_tensor(name, list(shape), F32, kind="ExternalInput").ap()

    def dscr(self, name, shape):
        kind = "ExternalOutput" if name in self.debug else "Internal"
        return self.nc.dram_tensor(name, list(shape), F32, kind=kind).ap()

    def sb(self, name, shape, dt=F32):
        return self.es.enter_context(self.nc.sbuf_tensor("sb_" + name, list(shape), dt))

    def wait(self, eng, sem, val):
        if val > 0:
            eng.wait_ge(sem.h, val)

    def inc(self, instr, sem, k=1):
        instr.then_inc(sem.h, k)
        sem.n += k
        return sem.n

    def note_store(self, sem):
        self.pending[id(sem)] = sem

    def phase_end(self, light=False):
        for s in self.pending.values():
            for e in ([self.POOL] if light else self.engs):
                self.wait(e, s, s.n)
        if not light:
            self.pending = {}
        self.nphase += 1
        if self.stop is not None and self.nphase >= self.stop:
            raise StopBuild()


def build(debug=(), stop=None):
    g = K(debug, stop)
    with g.es:
        try:
            _body(g)
        except StopBuild:
            pass
    return g


def _body(g):
    nc, es = g.nc, g.es
    PE, ACT, DVE, POOL, SP = g.PE, g.ACT, g.DVE, g.POOL, g.SP

    def ev(instr, sem, k=1):
        instr.then_inc(sem.h, k)
        sem.n += k
        return (sem, sem.n)

    def waitp(eng, pairs):
        if pairs is None:
            return
        if isinstance(pairs, tuple):
            pairs = [pairs]
        for (s_, v_) in pairs:
            if v_ > 0:
                eng.wait_ge(s_.h, v_)

    x_d = g.din("x", [T, D])
    ctx_d = g.din("ctx", [CT, D])
    c2T_d = g.din("c2T", [128, 64])
    bmodT_d = g.din("bmodT", [128, 192])
    gvec_d = g.din("gvec", [128, 160])
    cst_d = g.din("cst", [128, 768])
    wmod_d = g.din("w_mod", [D, 6 * D])
    win_d = g.din("w_in", [D, 4 * D])
    wfo_d = g.din("w_fourier", [2048, D])
    wna_d = g.din("w_na_out", [2048, D])
    wout_d = g.din("w_out", [D, D])
    wr_d = g.din("w_router", [D, NE])
    w1_d = g.din("w1", [NE, D, FF])
    w3_d = g.din("w3", [NE, D, FF])
    w2_d = g.din("w2", [NE, FF, D])
    cs_d = g.din("dft_cs", [512, 1024])
    cls_d = g.din("dft_cls", [2 * T, T])
    nab_d = g.din("nab", [NH_, 128, 14 * 64])
    out_d = nc.dram_tensor("out", [T, D], F32, kind="ExternalOutput").ap()

    S_xT = g.dscr("S_xT", [D, T])
    S_xnT = g.dscr("S_xnT", [D, T])
    S_cnT = g.dscr("S_cnT", [D, CT])
    S_ufT = g.dscr("S_ufT", [2048, T])
    S_qT = g.dscr("S_qT", [2048, T])
    S_kT = g.dscr("S_kT", [2048, T])
    S_v = g.dscr("S_v", [T, 2048])
    S_gT = g.dscr("S_gT", [2 * D, T])
    S_kcT = g.dscr("S_kcT", [2048, CT])
    S_vc = g.dscr("S_vc", [CT, 2048])
    S_AB = g.dscr("S_AB", [4, T, 1024])
    S_YT = g.dscr("S_YT", [2048, T])
    S_onT = g.dscr("S_onT", [2048, T])
    S_mT = g.dscr("S_mT", [D, T])
    S_x1T = g.dscr("S_x1T", [D, T])
    S_xn2T = g.dscr("S_xn2T", [D, T])
    S_xn2 = g.dscr("S_xn2", [T, D])
    S_selgT = g.dscr("S_selgT", [NE * CAP, T])
    S_eo = g.dscr("S_eo", [NE * CAP, D])
    S_x2T = g.dscr("S_x2T", [D, T])

    RB = g.sb("RB", [128, 16384], F32R)
    LR = [g.sb(f"LR{i}", [128, LSLOT], F32R) for i in range(NL)]
    FB = g.sb("FB", [128, 16384], F32)
    cst = g.sb("cst", [128, 768], F32)
    onesR = g.sb("onesR", [128, 128], F32R)
    tokR = g.sb("tokR", [128, 32], F32R)
    IDX = g.sb("idx", [128, 4], mybir.dt.int32)
    modT = g.sb("modT", [128, 192 * 2], F32)
    bmodT = g.sb("bmodT", [128, 192], F32)
    gvec = g.sb("gvec", [128, 160], F32)
    vec = g.sb("vec", [128, 10 * 32], F32)
    sT = g.sb("sT", [128, 64], F32R)
    c2T = g.sb("c2T", [128, 64], F32)
    small = g.sb("small", [128, 64], F32)
    rt = g.sb("rt", [128, 6 * 256], F32)
    rsm = g.sb("rsm", [128, 64], F32)
    affT = FB[0:16, 0:2 * T + 64]
    PS = es.enter_context(nc.psum_tensor("PS", [128, 8, 512], F32))

    ident = cst[:, 0:128]
    ones = cst[:, 128:256]
    tri = cst[:, 256:384]
    iota = cst[:, 384:640]

    VEC = {n: vec[:, i * 32:(i + 1) * 32] for i, n in enumerate(
        ["gs_m", "sh_m", "gs_mc", "sh_mc", "gt_m", "gs_f", "sh_f", "gt_f", "g_fin", "zero"])}
    bgate = gvec[:, 96:160]

    l_loaded = [g.sem(f"l_ld{i}") for i in range(NL)]
    l_free = [g.sem(f"l_fr{i}") for i in range(NL)]
    rb_loaded = g.sem("rb_ld")
    ps_free = [g.sem("ps_fr0"), g.sem("ps_fr1")]
    st_free = [g.sem(f"st_fr{i}") for i in range(NS)]
    ax_loaded = [[g.sem(f"ax_ld{k}_{i}") for i in range(NA)] for k in range(2)]
    s_misc = g.sem("misc")
    s_dve = g.sem("dve_ev")
    s_dve2 = g.sem("dve_ev2")
    s_act = g.sem("act_ev")
    s_pe = g.sem("pe_ev")
    s_x = [g.sem("x_ld0"), g.sem("x_ld1")]

    st = {"li": 0, "si": 0, "ai": [0, 0], "pset": 0, "cp": 0, "rb_busy": None}
    l_busy = [None] * NL
    ps_busy = [[], []]
    st_busy = [None] * NS
    ax_busy = [[None] * NA for _ in range(2)]
    STG = [FB[:, 12288 + i * 512: 12288 + (i + 1) * 512] for i in range(NS)]
    AUX = [[FB[:, 4096 + (k * NA + i) * 512: 4096 + (k * NA + i + 1) * 512] for i in range(NA)] for k in range(2)]

    def stage_acquire(eng):
        i = st["si"]; st["si"] += 1
        slot = i % NS
        waitp(eng, st_busy[slot])
        return slot

    def stage_store(slot, ready, dst_ap, src_ap=None):
        waitp(SP, ready)
        d = SP.dma_start(out=dst_ap, in_=STG[slot] if src_ap is None else src_ap)
        st_busy[slot] = ev(d, st_free[slot], 16)
        g.note_store(st_free[slot])

    def aux_load(kind, src_ap, w=512):
        i = st["ai"][kind]; st["ai"][kind] += 1
        slot = i % NA
        waitp(POOL, ax_busy[kind][slot])
        d = POOL.dma_start(out=AUX[kind][slot][:, 0:w], in_=src_ap)
        return (kind, slot, ev(d, ax_loaded[kind][slot], 16))

    def aux_use(eng, tok):
        waitp(eng, tok[2])
        return AUX[tok[0]][tok[1]]

    def aux_release(tok, pair):
        ax_busy[tok[0]][tok[1]] = pair

    def copy_eng():
        st["cp"] += 1
        return ACT if st["cp"] % 2 == 0 else DVE

    def evac_copy(eng, out, in_):
        if eng is ACT:
            return ACT.activation(out=out, in_=in_, func=AF.Copy)
        return DVE.tensor_copy(out=out, in_=in_)

    def ps_acquire():
        pset = st["pset"] % 2
        st["pset"] += 1
        waitp(PE, ps_busy[pset])
        ps_busy[pset] = []
        return pset

    def ps_rel(pset):
        def rel(instr):
            p = ev(instr, ps_free[pset])
            ps_busy[pset].append(p)
            return p
        return rel

    def ps_all():
        return list(ps_busy[0]) + list(ps_busy[1])

    def gemm(Kd, M, N, Lsrc, bank, Rsrc=None, Rres=None, MW=512, Nb=512, prep=None, r_wait=None):
        KT = Kd // 128
        KC = min(KT, LSLOT // MW)
        nkt = KT // KC
        W = min(512, Nb)
        NHh = max(1, Nb // 512)
        MC = MW // 128
        nb_banks = MC * NHh
        assert nb_banks <= 4 and KT % KC == 0 and M % MW == 0 and N % Nb == 0
        RBv = RB[:, 0:KT * Nb].rearrange("p (k n) -> p k n", k=KT) if Rres is None else None
        last = None
        for nb in range(N // Nb):
            n0 = nb * Nb
            rb_ready = None
            if Rres is None:
                waitp(POOL, st["rb_busy"])
                RKC = max(1, min(KT, 4096 // Nb))
                for kk in range(0, KT, RKC):
                    d = POOL.dma_start(out=RBv[:, kk:kk + RKC, :],
                                       in_=Rsrc(kk, RKC, n0, Nb).rearrange("(kc p) n -> p kc n", p=128))
                    rb_ready = ev(d, rb_loaded, 16)
            first = True
            for mg in range(M // MW):
                m0 = mg * MW
                ctx = prep(m0, n0) if prep is not None else None
                pset = ps_acquire()
                if first:
                    waitp(PE, rb_ready)
                    waitp(PE, r_wait)
                    first = False
                for kt in range(nkt):
                    i = st["li"]; st["li"] += 1
                    slot = i % NL
                    waitp(POOL, l_busy[slot])
                    Lt = LR[slot][:, 0:KC * MW].rearrange("p (k m) -> p k m", k=KC)
                    d = POOL.dma_start(out=Lt, in_=Lsrc(kt * KC, KC, m0, MW).rearrange("(kc p) m -> p kc m", p=128))
                    waitp(PE, ev(d, l_loaded[slot], 16))
                    mm = None
                    for kc in range(KC):
                        k = kt * KC + kc
                        for mc in range(MC):
                            for nh in range(NHh):
                                rhs = RBv[:, k, nh * W:(nh + 1) * W] if Rres is None else Rres(k, nh * W, W)
                                mm = PE.matmul(PS[:, pset * 4 + mc * NHh + nh, 0:W],
                                               lhsT=Lt[:, kc, mc * 128:(mc + 1) * 128], rhs=rhs,
                                               start=(k == 0), stop=(k == KT - 1))
                    last = ev(mm, l_free[slot])
                    l_busy[slot] = last
                if Rres is None:
                    st["rb_busy"] = last
                rel = ps_rel(pset)
                for mc in range(MC):
                    for nh in range(NHh):
                        bank(PS[:, pset * 4 + mc * NHh + nh, 0:W], m0 + mc * 128, n0 + nh * W, W, last, rel, ctx)
        return last

    def gemm_sr(Kd, M, N, Lres, Rsrc, bank, l_wait=None):
        KT = Kd // 128
        NW = 512
        KC = min(KT, LSLOT // NW)
        nkt = KT // KC
        MC = M // 128
        assert MC <= 4 and KT % KC == 0 and N % NW == 0
        first = True
        last = None
        for ng in range(N // NW):
            n0 = ng * NW
            pset = ps_acquire()
            if first:
                waitp(PE, l_wait)
                first = False
            for kt in range(nkt):
                i = st["li"]; st["li"] += 1
                slot = i % NL
                waitp(POOL, l_busy[slot])
                Rt = LR[slot][:, 0:KC * NW].rearrange("p (k n) -> p k n", k=KC)
                d = POOL.dma_start(out=Rt, in_=Rsrc(kt * KC, KC, n0, NW).rearrange("(kc p) n -> p kc n", p=128))
                waitp(PE, ev(d, l_loaded[slot], 16))
                mm = None
                for kc in range(KC):
                    k = kt * KC + kc
                    for mc in range(MC):
                        mm = PE.matmul(PS[:, pset * 4 + mc, 0:NW], lhsT=Lres(k, mc), rhs=Rt[:, kc, :],
                                       start=(k == 0), stop=(k == KT - 1))
                last = ev(mm, l_free[slot])
                l_busy[slot] = last
            rel = ps_rel(pset)
            for mc in range(MC):
                bank(PS[:, pset * 4 + mc, 0:NW], mc * 128, n0, NW, last, rel, None)
        return last

    def bank_copy_to(dst_fn):
        def bank(ps, mrow, n0, w, pe_wait, rel, ctx):
            eng = copy_eng()
            waitp(eng, pe_wait)
            slot = stage_acquire(eng)
            i = evac_copy(eng, STG[slot][:, 0:w], ps)
            stage_store(slot, rel(i), dst_fn(mrow, n0, w), STG[slot][:, 0:w])
        return bank

    ld = None
    for (dst, src) in [(cst, cst_d), (bmodT, bmodT_d), (gvec, gvec_d), (c2T, c2T_d)]:
        ld = ev(SP.dma_start(out=dst[:], in_=src), s_misc, 16)
    for e_ in (ACT, DVE, PE):
        waitp(e_, ld)
    ev(ACT.activation(out=sT[:], in_=c2T[:], func=AF.Silu), s_act)
    ev(ACT.activation(out=onesR[:], in_=ones, func=AF.Copy), s_act)
    sT_ready = ev(ACT.activation(out=tokR[:], in_=cst[:, 640:672], func=AF.Copy), s_act)

    modT3 = modT[:].rearrange("p (n j) -> p n j", j=2)

    def bank_mod(ps, mrow, n0, w, pe_wait, rel, ctx):
        waitp(DVE, pe_wait)
        ch = mrow // 128
        rel(DVE.tensor_scalar(out=modT3[:, ch, :], in0=ps, scalar1=bmodT[:, ch:ch + 1], scalar2=None, op0=ALU.add))

    gemm(D, 2 * D, 2, lambda k0, kn, m0, mw: wmod_d[k0 * 128:(k0 + kn) * 128, m0:m0 + mw], bank_mod,
         Rres=lambda k, n0, w: sT[:, 2 * k:2 * k + 2], MW=512, Nb=2, r_wait=sT_ready)
    waitp(DVE, ps_all())

    def mv(which, j):
        return modT3[:, which * 32:(which + 1) * 32, j]
    ops = [
        ("gs_m", lambda o: DVE.scalar_tensor_tensor(out=o, in0=mv(1, 0), scalar=1.0, in1=gvec[:, 0:32], op0=ALU.add, op1=ALU.mult)),
        ("sh_m", lambda o: DVE.tensor_copy(out=o, in_=mv(0, 0))),
        ("gs_mc", lambda o: DVE.scalar_tensor_tensor(out=o, in0=mv(1, 1), scalar=1.0, in1=gvec[:, 0:32], op0=ALU.add, op1=ALU.mult)),
        ("sh_mc", lambda o: DVE.tensor_copy(out=o, in_=mv(0, 1))),
        ("gt_m", lambda o: DVE.tensor_copy(out=o, in_=mv(2, 0))),
        ("gs_f", lambda o: DVE.scalar_tensor_tensor(out=o, in0=mv(4, 0), scalar=1.0, in1=gvec[:, 32:64], op0=ALU.add, op1=ALU.mult)),
        ("sh_f", lambda o: DVE.tensor_copy(out=o, in_=mv(3, 0))),
        ("gt_f", lambda o: DVE.tensor_copy(out=o, in_=mv(5, 0))),
        ("g_fin", lambda o: DVE.tensor_copy(out=o, in_=gvec[:, 64:96])),
    ]
    LATE = ("gt_m", "gs_f", "sh_f", "gt_f")
    for n_, f in ops:
        if n_ not in LATE:
            ev(f(VEC[n_]), s_dve)
    vec_ready = ev(DVE.memset(VEC["zero"], 0.0), s_dve)
    for e_ in (ACT, PE, POOL, SP, DVE):
        waitp(e_, vec_ready)

    XT = [FB[:, 0:4096], FB[:, 4096:8192]]
    XS = FB[:, 8192:12288]
    n_tiles = T // 128 + CT // 128
    xt_busy = [[], []]
    xs_busy = None
    for it in range(n_tiles):
        is_ctx = it >= T // 128
        src = ctx_d[(it - 16) * 128:(it - 15) * 128, :] if is_ctx else x_d[it * 128:(it + 1) * 128, :]
        xs_ = it % 2
        waitp(SP, xt_busy[xs_])
        xt_busy[xs_] = []
        x_ready = ev(SP.dma_start(out=XT[xs_], in_=src), s_x[xs_], 16)
        waitp(ACT, x_ready)
        waitp(ACT, xs_busy)
        sq = ev(ACT.activation(out=XS, in_=XT[xs_], func=AF.Square, accum_out=small[:, it:it + 1]), s_act)
        waitp(DVE, sq)
        p1 = ev(DVE.tensor_scalar(out=small[:, 32 + it:33 + it], in0=small[:, it:it + 1], scalar1=1.0 / D, scalar2=EPS, op0=ALU.mult, op1=ALU.add), s_dve)
        waitp(ACT, p1)
        pq = ev(ACT.activation(out=small[:, 32 + it:33 + it], in_=small[:, 32 + it:33 + it], func=AF.Sqrt), s_act)
        waitp(DVE, pq)
        p2 = ev(DVE.reciprocal(out=small[:, 32 + it:33 + it], in_=small[:, 32 + it:33 + it]), s_dve)
        waitp(DVE, p2)
        xs_ready = ev(DVE.tensor_scalar(out=XS, in0=XT[xs_], scalar1=small[:, 32 + it:33 + it], scalar2=None, op0=ALU.mult), s_dve)
        xt_busy[xs_].append(xs_ready)
        gs = VEC["gs_mc"] if is_ctx else VEC["gs_m"]
        sh = VEC["sh_mc"] if is_ctx else VEC["sh_m"]
        waitp(PE, x_ready)
        for kind in (0, 1):
            if kind == 0 and is_ctx:
                continue
            srcT = XT[xs_] if kind == 0 else XS
            if kind == 1:
                waitp(PE, xs_ready)
            for gq in range(8):
                pset = ps_acquire()
                tr = None
                for q in range(4):
                    c = gq * 4 + q
                    tr = PE.transpose(PS[:, pset * 4, q * 128:(q + 1) * 128], srcT[:, c * 128:(c + 1) * 128], ident)
                pe_done = ev(tr, s_pe)
                if gq == 7:
                    if kind == 0:
                        xt_busy[xs_].append(pe_done)
                    else:
                        xs_busy = pe_done
                eng = ACT if kind == 1 else copy_eng()
                waitp(eng, pe_done)
                slot = stage_acquire(eng)
                lasti = None
                if kind == 0:
                    lasti = evac_copy(eng, STG[slot], PS[:, pset * 4, :])
                    dst = S_xT[gq * 512:(gq + 1) * 512, it * 128:(it + 1) * 128]
                else:
                    for q in range(4):
                        c = gq * 4 + q
                        lasti = ACT.activation(out=STG[slot][:, q * 128:(q + 1) * 128], in_=PS[:, pset * 4, q * 128:(q + 1) * 128],
                                               func=AF.Identity, bias=sh[:, c:c + 1], scale=gs[:, c:c + 1])
                    if is_ctx:
                        dst = S_cnT[gq * 512:(gq + 1) * 512, (it - 16) * 128:(it - 15) * 128]
                    else:
                        dst = S_xnT[gq * 512:(gq + 1) * 512, it * 128:(it + 1) * 128]
                rdy = ps_rel(pset)(lasti)
                stage_store(slot, rdy, dst.rearrange("(q p) t -> p q t", p=128), STG[slot].rearrange("p (q t) -> p q t", q=4))
    g.phase_end()

    def win_dst(mrow, n0, w):
        if mrow < 2048:
            return S_ufT[mrow:mrow + 128, n0:n0 + w]
        if mrow < 4096:
            return S_qT[mrow - 2048:mrow - 2048 + 128, n0:n0 + w]
        return S_kT[mrow - 4096:mrow - 4096 + 128, n0:n0 + w]
    copy_win = bank_copy_to(win_dst)

    def bank_win(ps, mrow_, n0, w, pe_wait, rel, ctx):
        mrow = mrow_ if mrow_ < 6144 else mrow_ + 2048
        if mrow < 6144:
            return copy_win(ps, mrow, n0, w, pe_wait, rel, ctx)
        gi = (mrow - 8192) // 128
        waitp(ACT, pe_wait)
        slot = stage_acquire(ACT)
        i = ACT.activation(out=STG[slot][:, 0:w], in_=ps, func=AF.Sigmoid, bias=bgate[:, gi:gi + 1], scale=1.0)
        stage_store(slot, rel(i), S_gT[mrow - 8192:mrow - 8192 + 128, n0:n0 + w], STG[slot][:, 0:w])

    def win_L(k0, kn, m0, mw):
        col = m0 if m0 < 6144 else m0 + 2048
        return win_d[k0 * 128:(k0 + kn) * 128, col:col + mw]

    gemm(D, 6144 + 8192, T, win_L, bank_win,
         Rsrc=lambda k0, kn, n0, nw: S_xnT[k0 * 128:(k0 + kn) * 128, n0:n0 + nw], MW=512, Nb=512)
    gemm(D, T, 2048, lambda k0, kn, m0, mw: S_xnT[k0 * 128:(k0 + kn) * 128, m0:m0 + mw],
         bank_copy_to(lambda mrow, n0, w: S_v[mrow:mrow + 128, n0:n0 + w]),
         Rsrc=lambda k0, kn, n0, nw: win_d[k0 * 128:(k0 + kn) * 128, 6144 + n0:6144 + n0 + nw], MW=512, Nb=512)
    gemm(D, 2048, CT, lambda k0, kn, m0, mw: win_d[k0 * 128:(k0 + kn) * 128, 4096 + m0:4096 + m0 + mw],
         bank_copy_to(lambda mrow, n0, w: S_kcT[mrow:mrow + 128, n0:n0 + w]),
         Rsrc=lambda k0, kn, n0, nw: S_cnT[k0 * 128:(k0 + kn) * 128, n0:n0 + nw], MW=512, Nb=256)
    gemm(D, CT, 2048, lambda k0, kn, m0, mw: S_cnT[k0 * 128:(k0 + kn) * 128, m0:m0 + mw],
         bank_copy_to(lambda mrow, n0, w: S_vc[mrow:mrow + 128, n0:n0 + w]),
         Rsrc=lambda k0, kn, n0, nw: win_d[k0 * 128:(k0 + kn) * 128, 6144 + n0:6144 + n0 + nw], MW=256, Nb=512)
    g.phase_end(light=True)

    for gi in range(4):
        gemm(512, T, 1024, lambda k0, kn, m0, mw, gi=gi: S_ufT[gi * 512 + k0 * 128: gi * 512 + (k0 + kn) * 128, m0:m0 + mw],
             bank_copy_to(lambda mrow, n0, w, gi=gi: S_AB[gi, mrow:mrow + 128, n0:n0 + w]),
             Rsrc=lambda k0, kn, n0, nw: cs_d[k0 * 128:(k0 + kn) * 128, n0:n0 + nw], MW=256, Nb=1024)
    g.phase_end(light=True)
    def L_ab(k0, kn, m0, mw):
        gi, mo = m0 // 512, m0 % 512
        if k0 < 16:
            return S_AB[gi, k0 * 128:(k0 + kn) * 128, mo:mo + mw]
        return S_AB[gi, (k0 - 16) * 128:(k0 - 16 + kn) * 128, 512 + mo:512 + mo + mw]
    gemm(2 * T, 2048, T, L_ab,
         bank_copy_to(lambda mrow, n0, w: S_YT[mrow:mrow + 128, n0:n0 + w]),
         Rsrc=lambda k0, kn, n0, nw: cls_d[k0 * 128:(k0 + kn) * 128, n0:n0 + nw], MW=512, Nb=512)
    g.phase_end(light=True)

    def prep_aux(srcs, MC=4, NHh=1):
        def prep(m0, n0):
            toks = {}
            for mc in range(MC):
                for nh in range(NHh):
                    toks[(m0 + mc * 128, n0 + nh * 512)] = [
                        aux_load(k, s_[off + m0 + mc * 128: off + m0 + (mc + 1) * 128, n0 + nh * 512:n0 + (nh + 1) * 512])
                        for k, (s_, off) in enumerate(srcs)]
            return toks
        return prep

    def bank_mul_gate(ps, mrow, n0, w, pe_wait, rel, ctx):
        tk = ctx[(mrow, n0)][0]
        waitp(DVE, pe_wait)
        a = aux_use(DVE, tk)
        slot = stage_acquire(DVE)
        p = rel(DVE.tensor_tensor(out=STG[slot][:, 0:w], in0=ps, in1=a[:, 0:w], op=ALU.mult))
        aux_release(tk, p)
        stage_store(slot, p, S_mT[mrow:mrow + 128, n0:n0 + w], STG[slot][:, 0:w])

    gemm(2048, D, T, lambda k0, kn, m0, mw: wfo_d[k0 * 128:(k0 + kn) * 128, m0:m0 + mw], bank_mul_gate,
         Rsrc=lambda k0, kn, n0, nw: S_YT[k0 * 128:(k0 + kn) * 128, n0:n0 + nw], MW=256, Nb=1024, prep=prep_aux([(S_gT, 0)], 2, 2))
    g.phase_end()

    qT = RB[:, 0:2048]
    kT = RB[:, 2048:4096]
    kcT = RB[:, 4096:4352]
    Vev = RB[:, 4352:6400].rearrange("p (c d) -> p c d", c=16)
    Vod = RB[:, 6400:8448].rearrange("p (c d) -> p c d", c=16)
    Vc = RB[:, 8448:8704].rearrange("p (c d) -> p c d", c=2)
    PT = [RB[:, 8704:9088], RB[:, 9088:9472], RB[:, 9472:9856]]
    TT = FB[:, 0:896].rearrange("p (d c) -> p d c", d=14)
    Sb = [FB[:, 1024:1280], FB[:, 1280:1536], FB[:, 6656:6912]]
    rden = [FB[:, 1536:1600], FB[:, 1600:1664]]
    onT = [FB[:, 2048:4096], FB[:, 4096:6144]]
    a_ld = g.sem("a_ld"); a_s = g.sem("a_s"); a_sb = g.sem("a_sb"); a_pt = g.sem("a_pt"); a_o = g.sem("a_o")
    a_dv = g.sem("a_dv"); a_on = g.sem("a_on")
    P_s = {}; P_sb = {}; P_pt = {}; P_o = {}; P_dv = {}; P_r0 = {}
    on_store = {}
    def deferred_mod():
        ROW = FB[0:2, 8192:8704]
        b6_busy = None
        b7_busy = None
        row_busy = None
        for ng in range(32):
            col0 = 2 * D + ng * 512
            waitp(PE, b6_busy)
            last = None
            for kt in range(4):
                i_ = st["li"]; st["li"] += 1
                slot = i_ % NL
                waitp(POOL, l_busy[slot])
                Rt = LR[slot][:, 0:4096].rearrange("p (k n) -> p k n", k=8)
                d = POOL.dma_start(out=Rt, in_=wmod_d[kt * 1024:(kt + 1) * 1024, col0:col0 + 512].rearrange("(kc p) n -> p kc n", p=128))
                waitp(PE, ev(d, l_loaded[slot], 16))
                mm = None
                for kc in range(8):
                    k = kt * 8 + kc
                    mm = PE.matmul(PS[0:2, 6, 0:512], lhsT=sT[:, 2 * k:2 * k + 2], rhs=Rt[:, kc, :], start=(k == 0), stop=(k == 31))
                last = ev(mm, l_free[slot])
                l_busy[slot] = last
                yield
            waitp(ACT, last)
            waitp(ACT, row_busy)
            cpy = ev(ACT.activation(out=ROW, in_=PS[0:2, 6, 0:512], func=AF.Copy), s_act)
            b6_busy = cpy
            waitp(PE, cpy)
            waitp(PE, b7_busy)
            tr = None
            for q in range(4):
                tr = PE.transpose(PS[:, 7, 2 * q:2 * q + 2], ROW[:, q * 128:(q + 1) * 128], cst[0:2, 0:2])
            trp = ev(tr, s_pe)
            row_busy = trp
            waitp(DVE, trp)
            ii = None
            for q in range(4):
                ch = col0 // 128 + q
                ii = DVE.tensor_scalar(out=modT3[:, ch, :], in0=PS[:, 7, 2 * q:2 * q + 2], scalar1=bmodT[:, ch:ch + 1], scalar2=None, op0=ALU.add)
            b7_busy = ev(ii, s_dve2)
            st["mod_done"] = b7_busy
            yield

    dgen = deferred_mod()
    dstep = 0
    itg = 0
    for h in range(NH_):
        hs = slice(h * 128, (h + 1) * 128)
        waitp(POOL, P_o.get(itg - 1))
        ldp = None
        for (dst, src) in [(qT, S_qT[hs, :]), (kT, S_kT[hs, :]), (kcT, S_kcT[hs, :]),
                           (Vev, S_v[:, hs].rearrange("(c p) d -> p c d", p=128)),
                           (Vod[:, 0:15, :], S_v[64:64 + 15 * 128, hs].rearrange("(c p) d -> p c d", p=128)),
                           (Vc, S_vc[:, hs].rearrange("(c p) d -> p c d", p=128))]:
            ldp = ev(POOL.dma_start(out=dst, in_=src), a_ld, 16)
        waitp(POOL, P_sb.get(itg - 1))
        ldp = ev(POOL.dma_start(out=FB[:, 0:896], in_=nab_d[h]), a_ld, 16)
        waitp(PE, ldp)
        waitp(DVE, ldp)
        ob = h % 2
        waitp(DVE, on_store.get(h - 2))
        def stage_S(i, r):
            rs = min(max(r - 4, 0), 24)
            d0 = rs - r + 7
            b = i % 3
            waitp(PE, P_pt.get(i - 3))
            waitp(PE, P_sb.get(i - 3))
            qv = qT[:, r * 64:(r + 1) * 64]
            mm = None
            for j in range(4):
                t0 = (rs + 2 * j) * 64
                mm = PE.matmul(PS[:, b, j * 64:(j + 1) * 64], lhsT=kT[:, t0:t0 + 128], rhs=qv, start=True, stop=True)
            for c in range(2):
                mm = PE.matmul(PS[:, b, 256 + c * 64:256 + (c + 1) * 64], lhsT=kcT[:, c * 128:(c + 1) * 128], rhs=qv, start=True, stop=True)
            P_s[i] = ev(mm, a_s)
            waitp(DVE, P_s[i])
            waitp(DVE, P_pt.get(i - 3))
            ii = DVE.scalar_tensor_tensor(out=Sb[b].rearrange("p (j c) -> p j c", j=4), in0=PS[:, b, 0:256].rearrange("p (j c) -> p j c", j=4),
                                          scalar=SCALE, in1=TT[:, d0:d0 + 7:2, :], op0=ALU.mult, op1=ALU.add)
            P_sb[i] = ev(ii, a_sb)
            waitp(ACT, P_sb[i])
            waitp(ACT, P_o.get(i - 3))
            ACT.activation(out=PT[b][:, 0:256], in_=Sb[b], func=AF.Exp)
            e2 = ACT.activation(out=PT[b][:, 256:384], in_=PS[:, b, 256:384], func=AF.Exp, scale=SCALE)
            P_pt[i] = ev(e2, a_pt)

        def stage_PV(i, r):
            rs = min(max(r - 4, 0), 24)
            b = i % 2
            s3 = i % 3
            waitp(PE, P_pt[i])
            waitp(PE, P_dv.get(i - 2))
            par = rs % 2
            for j in range(4):
                rowp = rs + 2 * j
                vt = Vev[:, rowp // 2, :] if par == 0 else Vod[:, (rowp - 1) // 2, :]
                PE.matmul(PS[:, 3 + b, 0:64], lhsT=vt, rhs=PT[s3][:, j * 64:(j + 1) * 64], start=(j == 0), stop=False)
            for c in range(2):
                PE.matmul(PS[:, 3 + b, 0:64], lhsT=Vc[:, c, :], rhs=PT[s3][:, 256 + c * 64:256 + (c + 1) * 64], start=False, stop=(c == 1))
            waitp(PE, P_r0.get(i - 1))
            mm = PE.matmul(PS[:, 5, 0:384], lhsT=onesR[:], rhs=PT[s3][:, 0:384], start=True, stop=True)
            P_o[i] = ev(mm, a_o)
            waitp(DVE, P_o[i])
            pr0 = ev(DVE.tensor_reduce(out=rden[b], in_=PS[:, 5, 0:384].rearrange("p (j q) -> p q j", j=6), axis=AXL.X, op=ALU.add), a_dv)
            P_r0[i] = pr0
            waitp(DVE, pr0)
            pr = ev(DVE.reciprocal(out=rden[b], in_=rden[b]), a_dv)
            waitp(DVE, pr)
            i2 = DVE.tensor_tensor(out=onT[ob][:, r * 64:(r + 1) * 64], in0=PS[:, 3 + b, 0:64], in1=rden[b], op=ALU.mult)
            P_dv[i] = ev(i2, a_dv)

        for r in range(34):
            if r < 32:
                stage_S(itg + r, r)
            if r >= 2:
                stage_PV(itg + r - 2, r - 2)
            dstep += 1
            if dstep % 3 == 0:
                next(dgen, None)
        itg += 32
        waitp(SP, P_dv[itg - 1])
        on_store[h] = ev(SP.dma_start(out=S_onT[hs, :], in_=onT[ob]), a_on, 16)
    g.note_store(a_on)
    for _ in dgen:
        pass
    waitp(DVE, st["mod_done"])
    late_ready = None
    for n_, f in ops:
        if n_ in LATE:
            late_ready = ev(f(VEC[n_]), s_dve)
    for e_ in (ACT, PE, POOL, SP, DVE):
        waitp(e_, late_ready)
    g.phase_end()

    def bank_gate_add(ps, mrow, n0, w, pe_wait, rel, ctx):
        tk, tk2 = ctx[(mrow, n0)]
        waitp(DVE, pe_wait)
        a = aux_use(DVE, tk)
        slot = stage_acquire(DVE)
        p = rel(DVE.tensor_tensor(out=STG[slot][:, 0:w], in0=ps, in1=a[:, 0:w], op=ALU.mult))
        aux_release(tk, p)
        waitp(DVE, p)
        a2 = aux_use(DVE, tk2)
        p2 = ev(DVE.tensor_tensor(out=STG[slot][:, 0:w], in0=STG[slot][:, 0:w], in1=a2[:, 0:w], op=ALU.add), s_dve2)
        aux_release(tk2, p2)
        stage_store(slot, p2, S_mT[mrow:mrow + 128, n0:n0 + w], STG[slot][:, 0:w])

    gemm(2048, D, T, lambda k0, kn, m0, mw: wna_d[k0 * 128:(k0 + kn) * 128, m0:m0 + mw], bank_gate_add,
         Rsrc=lambda k0, kn, n0, nw: S_onT[k0 * 128:(k0 + kn) * 128, n0:n0 + nw], MW=256, Nb=1024, prep=prep_aux([(S_gT, D), (S_mT, 0)], 2, 2))
    g.phase_end(light=True)

    def bank_res(gt, dst):
        def bank(ps, mrow, n0, w, pe_wait, rel, ctx):
            tk = ctx[(mrow, n0)][0]
            ch = mrow // 128
            waitp(DVE, pe_wait)
            a = aux_use(DVE, tk)
            slot = stage_acquire(DVE)
            p = rel(DVE.scalar_tensor_tensor(out=STG[slot][:, 0:w], in0=ps, scalar=gt[:, ch:ch + 1], in1=a[:, 0:w], op0=ALU.mult, op1=ALU.add))
            aux_release(tk, p)
            stage_store(slot, p, dst[mrow:mrow + 128, n0:n0 + w], STG[slot][:, 0:w])
        return bank

    gemm(D, D, T, lambda k0, kn, m0, mw: wout_d[k0 * 128:(k0 + kn) * 128, m0:m0 + mw], bank_res(VEC["gt_m"], S_x1T),
         Rsrc=lambda k0, kn, n0, nw: S_mT[k0 * 128:(k0 + kn) * 128, n0:n0 + nw], MW=512, Nb=512, prep=prep_aux([(S_xT, 0)]))
    g.phase_end()

    n_ld = [g.sem(f"n_ld{i}") for i in range(4)]
    n_sq = g.sem("n_sq"); n_mm = g.sem("n_mm"); n_d1 = g.sem("n_d1"); n_a = g.sem("n_a"); n_tr = g.sem("n_tr"); n_cp = g.sem("n_cp")
    n_st = [g.sem("n_st0"), g.sem("n_st1")]; n_tm = [g.sem("n_tm0"), g.sem("n_tm1")]

    def norm_phase(srcT, gs, sh, dstT, dst_tm):
        XC = [FB[:, i * 512:(i + 1) * 512] for i in range(4)]
        SQ = [FB[:, 2048 + i * 512: 2048 + (i + 1) * 512] for i in range(2)]
        RS = FB[:, 3072:3584]
        TMP2 = [FB[:, 3584:4096], FB[:, 6144:6656]]
        TB = [FB[:, 4096:4608], FB[:, 4608:5120]]
        NT = [FB[:, 5120:5632], FB[:, 5632:6144]]
        xc_busy = [None] * 4
        sq_busy = [None, None]
        nt_busy = [[], []]
        tb_busy = [None, None]
        bank_busy = {4: None, 5: None}
        tmp_busy = [None, None]
        ps0_busy = None
        li = 0; sj = 0; tc = 0
        for tb in range(T // 512):
            t0 = tb * 512
            mmp = None
            for c in range(32):
                slot = li % 4; li += 1
                waitp(POOL, xc_busy[slot])
                ldp = ev(POOL.dma_start(out=XC[slot], in_=srcT[c * 128:(c + 1) * 128, t0:t0 + 512]), n_ld[slot], 16)
                waitp(ACT, ldp)
                sb_ = sj % 2; sj += 1
                waitp(ACT, sq_busy[sb_])
                ap_ = ev(ACT.activation(out=SQ[sb_], in_=XC[slot], func=AF.Square), n_sq)
                xc_busy[slot] = ap_
                waitp(PE, ap_)
                if c == 0:
                    waitp(PE, ps0_busy)
                mmp = ev(PE.matmul(PS[:, 0, :], lhsT=ones, rhs=SQ[sb_], start=(c == 0), stop=(c == 31)), n_mm)
                sq_busy[sb_] = mmp
            waitp(DVE, mmp)
            p1 = ev(DVE.tensor_scalar(out=RS, in0=PS[:, 0, :], scalar1=1.0 / D, scalar2=EPS, op0=ALU.mult, op1=ALU.add), s_dve)
            ps0_busy = p1
            waitp(ACT, p1)
            pq = ev(ACT.activation(out=RS, in_=RS, func=AF.Sqrt), s_act)
            waitp(DVE, pq)
            p2 = ev(DVE.reciprocal(out=RS, in_=RS), s_dve)
            waitp(DVE, p2)
            pend = {}

            def stage_A(c):
                nonlocal li, tc
                slot = li % 4; li += 1
                waitp(POOL, xc_busy[slot])
                ldp = ev(POOL.dma_start(out=XC[slot], in_=srcT[c * 128:(c + 1) * 128, t0:t0 + 512]), n_ld[slot], 16)
                nb_ = tc % 2
                waitp(DVE, ldp)
                waitp(DVE, tmp_busy[nb_])
                d1 = ev(DVE.tensor_tensor(out=TMP2[nb_], in0=XC[slot], in1=RS, op=ALU.mult), n_d1)
                xc_busy[slot] = d1
                waitp(ACT, d1)
                waitp(ACT, nt_busy[nb_])
                ap_ = ev(ACT.activation(out=NT[nb_], in_=TMP2[nb_], func=AF.Identity, bias=sh[:, c:c + 1], scale=gs[:, c:c + 1]), n_a)
                tmp_busy[nb_] = ap_
                bk = 4 + nb_
                waitp(PE, ap_)
                waitp(PE, bank_busy[bk])
                tr = None
                for q in range(4):
                    tr = PE.transpose(PS[:, bk, q * 128:(q + 1) * 128], NT[nb_][:, q * 128:(q + 1) * 128], ident)
                trp = ev(tr, n_tr)
                nt_busy[nb_] = [trp]
                if dstT is not None:
                    waitp(SP, ap_)
                    sp_ = ev(SP.dma_start(out=dstT[c * 128:(c + 1) * 128, t0:t0 + 512], in_=NT[nb_]), n_st[nb_], 16)
                    nt_busy[nb_].append(sp_)
                    g.note_store(n_st[nb_])
                pend[c] = (nb_, bk, trp)
                tc += 1

            def stage_B(c):
                nb_, bk, trp = pend.pop(c)
                waitp(DVE, trp)
                waitp(DVE, tb_busy[nb_])
                cp = ev(DVE.tensor_copy(out=TB[nb_], in_=PS[:, bk, :]), n_cp)
                bank_busy[bk] = cp
                waitp(SP, cp)
                tb_busy[nb_] = ev(SP.dma_start(out=dst_tm[t0:t0 + 512, c * 128:(c + 1) * 128].rearrange("(q p) d -> p q d", p=128),
                                               in_=TB[nb_].rearrange("p (q d) -> p q d", q=4)), n_tm[nb_], 16)
                g.note_store(n_tm[nb_])

            for c in range(33):
                if c < 32:
                    stage_A(c)
                if c >= 1:
                    stage_B(c - 1)

    norm_phase(S_x1T, VEC["gs_f"], VEC["sh_f"], S_xn2T, S_xn2)
    g.phase_end()

    rt3 = rt[:].rearrange("p (a i e) -> p a i e", a=6, i=16)
    LG, EX, AFF, MASK, GM, RANK = [rt3[:, a] for a in range(6)]

    def bank_router(ps, mrow, n0, w, pe_wait, rel, ctx):
        waitp(DVE, pe_wait)
        rel(DVE.tensor_copy(out=LG[:, mrow // 128, :], in_=ps))

    gemm(D, T, NE, lambda k0, kn, m0, mw: S_xn2T[k0 * 128:(k0 + kn) * 128, m0:m0 + mw], bank_router,
         Rsrc=lambda k0, kn, n0, nw: wr_d[k0 * 128:(k0 + kn) * 128, n0:n0 + nw], MW=512, Nb=NE)
    waitp(DVE, ps_all())
    waitp(PE, ps_all())
    MX = rsm[:, 0:16]; SM = rsm[:, 16:32]; RSM = rsm[:, 32:48]; THRB = rsm[:, 48:64]

    def dchain(i):
        p = ev(i, s_dve)
        waitp(DVE, p)
        return p
    dchain(DVE.tensor_reduce(out=MX, in_=LG, axis=AXL.X, op=ALU.max))
    p = dchain(DVE.tensor_scalar(out=MX, in0=MX, scalar1=-1.0, scalar2=None, op0=ALU.mult))
    waitp(ACT, p)
    a = None
    for tI in range(16):
        a = ACT.activation(out=EX[:, tI, :], in_=LG[:, tI, :], func=AF.Exp, bias=MX[:, tI:tI + 1], scale=1.0, accum_out=SM[:, tI:tI + 1])
    waitp(DVE, ev(a, s_act))
    dchain(DVE.reciprocal(out=RSM, in_=SM))
    i = None
    for tI in range(16):
        i = DVE.tensor_scalar(out=AFF[:, tI, :], in0=EX[:, tI, :], scalar1=RSM[:, tI:tI + 1], scalar2=None, op0=ALU.mult)
    p = dchain(i)
    waitp(PE, p)
    tr = None
    for tI in range(16):
        tr = PE.transpose(PS[0:16, tI // 4, (tI % 4) * 128:(tI % 4 + 1) * 128], AFF[:, tI, :], ident)
    waitp(DVE, ev(tr, s_pe))
    WK = affT[:, T:2 * T]; M8 = affT[:, 2 * T:2 * T + 8]; M8b = affT[:, 2 * T + 8:2 * T + 16]
    THR = affT[:, 2 * T + 16:2 * T + 17]
    for bq in range(4):
        i = DVE.tensor_copy(out=WK[:, bq * 512:(bq + 1) * 512], in_=PS[0:16, bq, :])
    dchain(i)
    for rr in range(CAP // 8):
        dchain(DVE.max(out=M8, in_=WK))
        dchain(DVE.match_replace(out=WK, in_to_replace=M8, in_values=WK, imm_value=-1.0))
    dchain(DVE.max(out=M8b, in_=WK))
    dchain(DVE.tensor_tensor(out=THR, in0=M8[:, 7:8], in1=M8b[:, 0:1], op=ALU.add))
    dchain(DVE.tensor_scalar(out=THR, in0=THR, scalar1=0.5, scalar2=None, op0=ALU.mult))
    THB = affT[:, 0:128]
    p = dchain(DVE.tensor_scalar(out=THB, in0=cst[0:16, 128:256], scalar1=THR, scalar2=None, op0=ALU.mult))
    waitp(PE, p)
    mm = PE.matmul(PS[:, 4, 0:16], lhsT=THB, rhs=cst[0:16, 0:16], start=True, stop=True)
    waitp(DVE, ev(mm, s_pe))
    dchain(DVE.tensor_copy(out=THRB, in_=PS[:, 4, 0:16]))
    for tI in range(16):
        i = DVE.tensor_tensor(out=MASK[:, tI, :], in0=AFF[:, tI, :], in1=THRB, op=ALU.is_ge)
    dchain(i)
    p = dchain(DVE.tensor_tensor(out=GM, in0=AFF, in1=MASK, op=ALU.mult))
    waitp(PE, p)
    for tI in range(16):
        for jj in range(tI):
            PE.matmul(PS[:, 5, tI * 16:(tI + 1) * 16], lhsT=ones, rhs=MASK[:, jj, :], start=(jj == 0), stop=False)
        mm = PE.matmul(PS[:, 5, tI * 16:(tI + 1) * 16], lhsT=tri, rhs=MASK[:, tI, :], start=(tI == 0), stop=True)
    waitp(DVE, ev(mm, s_pe))
    p = dchain(DVE.tensor_copy(out=RANK, in_=PS[:, 5, 0:256].rearrange("p (i e) -> p i e", i=16)))
    for e_ in (ACT, PE, POOL, SP):
        waitp(e_, p)

    xinT = RB[:, 0:8192].rearrange("p (k n) -> p k n", k=32)
    hidT = RB[:, 8192:12288].rearrange("p (k n) -> p k n", k=16)
    SEL = RB[:, 12288:16384].rearrange("p (k n) -> p k n", k=16)
    H1 = FB[:, 0:4096].rearrange("p (k n) -> p k n", k=16)
    SG = [FB[:, 15360:15616], FB[:, 15616:15872]]
    e_sel = g.sem("e_sel"); e_sg = g.sem("e_sg"); e_tr = g.sem("e_tr"); e_cp = g.sem("e_cp")
    sg_busy = [None, None]
    bk_busy = [None, None]
    XIN = [FB[:, 4096:8192], FB[:, 8192:12288]]
    e_gl = g.sem("e_gl")
    xin_busy = None
    sgc = 0
    gath_done = None
    w2_done = None
    for e in range(NE):
        waitp(DVE, gath_done)
        i = None
        for tI in range(16):
            i = DVE.tensor_scalar(out=SEL[:, tI, :], in0=iota, scalar1=RANK[:, tI, e:e + 1], scalar2=MASK[:, tI, e:e + 1],
                                  op0=ALU.is_equal, op1=ALU.mult)
        sel_ready = ev(i, e_sel)
        waitp(PE, ps_all())
        for tI in range(16):
            b2 = sgc % 2
            waitp(DVE, sg_busy[b2])
            sp_ = ev(DVE.tensor_scalar(out=SG[b2], in0=iota, scalar1=RANK[:, tI, e:e + 1], scalar2=GM[:, tI, e:e + 1],
                                       op0=ALU.is_equal, op1=ALU.mult), e_sg)
            waitp(PE, sp_)
            waitp(PE, bk_busy[b2])
            tr = None
            for hh in range(2):
                tr = PE.transpose(PS[:, 6 + b2, hh * 128:(hh + 1) * 128], SG[b2][:, hh * 128:(hh + 1) * 128], ident)
            trp = ev(tr, e_tr)
            sg_busy[b2] = trp
            waitp(ACT, trp)
            slot = stage_acquire(ACT)
            cp = ev(ACT.activation(out=STG[slot][:, 0:256], in_=PS[:, 6 + b2, 0:256], func=AF.Copy), e_cp)
            bk_busy[b2] = cp
            stage_store(slot, cp, S_selgT[e * 256:(e + 1) * 256, tI * 128:(tI + 1) * 128].rearrange("(h p) t -> p h t", p=128),
                        STG[slot][:, 0:256].rearrange("p (h t) -> p h t", h=2))
            sgc += 1
        waitp(PE, bk_busy[0]); waitp(PE, bk_busy[1])

        waitp(PE, sel_ready)
        mm = None
        for hh in range(2):
            for tI in range(16):
                mm = PE.matmul(PS[:, 5, 2 * hh:2 * hh + 2], lhsT=SEL[:, tI, hh * 128:(hh + 1) * 128], rhs=tokR[:, 2 * tI:2 * tI + 2],
                               start=(tI == 0), stop=(tI == 15))
        gath_done = ev(mm, e_tr)
        waitp(DVE, gath_done)
        pconv = ev(DVE.tensor_copy(out=IDX[:], in_=PS[:, 5, 0:4]), e_sg)
        waitp(POOL, pconv)
        waitp(POOL, xin_busy)
        pg = []
        for hh in range(2):
            dd = POOL.indirect_dma_start(out=XIN[hh], out_offset=None, in_=S_xn2[:, :],
                                         in_offset=bass.IndirectOffsetOnAxis(ap=IDX[:, 2 * hh:2 * hh + 1].bitcast(mybir.dt.uint32), axis=0))
            pg.append(ev(dd, e_gl, 16))
        for hh in range(2):
            waitp(PE, pg[hh])
            for cg in range(8):
                pset = ps_acquire()
                tr = None
                for q in range(4):
                    c = cg * 4 + q
                    tr = PE.transpose(PS[:, pset * 4, q * 128:(q + 1) * 128], XIN[hh][:, c * 128:(c + 1) * 128], ident)
                trp = ev(tr, e_tr)
                xin_busy = trp
                eng = copy_eng()
                waitp(eng, trp)
                ps_rel(pset)(evac_copy(eng, xinT[:, cg * 4:(cg + 1) * 4, hh * 128:(hh + 1) * 128],
                                       PS[:, pset * 4, :].rearrange("p (q s) -> p q s", q=4)))
        x_ready = ps_all()

        def bank_h1(ps, mrow, n0, w, pe_wait, rel, ctx):
            waitp(ACT, pe_wait)
            rel(ACT.activation(out=H1[:, mrow // 128, :], in_=ps, func=AF.Silu))
        gemm(D, FF, CAP, lambda k0, kn, m0, mw, e=e: w1_d[e, k0 * 128:(k0 + kn) * 128, m0:m0 + mw], bank_h1,
             Rres=lambda k, n0, w: xinT[:, k, :], MW=512, Nb=CAP, r_wait=x_ready)
        h1_ready = ps_all()

        def bank_hid(ps, mrow, n0, w, pe_wait, rel, ctx):
            waitp(DVE, pe_wait)
            rel(DVE.tensor_tensor(out=hidT[:, mrow // 128, :], in0=ps, in1=H1[:, mrow // 128, :], op=ALU.mult))
        waitp(DVE, h1_ready)
        waitp(DVE, w2_done)
        gemm(D, FF, CAP, lambda k0, kn, m0, mw, e=e: w3_d[e, k0 * 128:(k0 + kn) * 128, m0:m0 + mw], bank_hid,
             Rres=lambda k, n0, w: xinT[:, k, :], MW=512, Nb=CAP)
        hid_ready = ps_all()
        waitp(ACT, hid_ready)
        w2_done = gemm_sr(FF, CAP, D, lambda k, mc: hidT[:, k, mc * 128:(mc + 1) * 128],
                          lambda k0, kn, n0, nw, e=e: w2_d[e, k0 * 128:(k0 + kn) * 128, n0:n0 + nw],
                          bank_copy_to(lambda mrow, n0, w, e=e: S_eo[e * 256 + mrow: e * 256 + mrow + 128, n0:n0 + w]),
                          l_wait=hid_ready)
    g.phase_end()

    gemm(NE * CAP, D, T, lambda k0, kn, m0, mw: S_eo[k0 * 128:(k0 + kn) * 128, m0:m0 + mw], bank_res(VEC["gt_f"], S_x2T),
         Rsrc=lambda k0, kn, n0, nw: S_selgT[k0 * 128:(k0 + kn) * 128, n0:n0 + nw], MW=512, Nb=512, prep=prep_aux([(S_x1T, 0)]))
    g.phase_end()

    norm_phase(S_x2T, VEC["g_fin"], VEC["zero"], None, out_d)
    g.phase_end()


_NC_CACHE = {}


def _consts():
    cst = np.zeros((128, 768), np.float32)
    cst[:, 0:128] = np.eye(128, dtype=np.float32)
    cst[:, 128:256] = 1.0
    cst[:, 256:384] = np.triu(np.ones((128, 128), np.float32), k=1)
    cst[:, 384:640] = np.arange(256, dtype=np.float32)[None, :]
    cst[:, 640:672] = (np.repeat(np.arange(16), 2)[None, :] * 128 + np.arange(128)[:, None]).astype(np.float32)
    ch = np.arange(512, dtype=np.int64)
    ang = 2.0 * np.pi * ((ch[:, None] * ch[None, :]) % 512).astype(np.float64) / 512.0
    cs = np.concatenate([np.cos(ang), np.sin(ang)], axis=1) / 1024.0
    t = np.arange(T, dtype=np.int64)
    angl = 2.0 * np.pi * ((t[:, None] * t[None, :]) % T).astype(np.float64) / T
    cls = np.concatenate([np.cos(angl), -np.sin(angl)], axis=0)
    return cst, cs.astype(np.float32), cls.astype(np.float32)


def _fm(v, n):
    return np.ascontiguousarray(np.asarray(v, np.float32).reshape(n, 128).T)


def _bias_tables(rel_bias):
    c = np.arange(64)
    kc = np.arange(64)
    win = np.clip(c - 8, 0, 48)
    rel = kc[:, None] - win[None, :]
    mask = (rel >= 0) & (rel < 16)
    dc = np.clip(kc[:, None] - c[None, :] + 15, 0, 30)
    H = rel_bias.shape[0]
    out = np.full((H, 2, 64, 14, 64), NEG, np.float32)
    for a in range(2):
        for d in range(14):
            vals = rel_bias[:, d + a][:, dc]
            out[:, a, :, d, :] = np.where(mask[None], vals, np.float32(NEG))
    return np.ascontiguousarray(out.reshape(H, 128, 14 * 64))


def kernel(x, c, ctx, c_ctx, w_mod, b_mod, norm_mix_g, w_in, b_gate, w_fourier, na_rel_bias,
           w_na_out, w_out, norm_ffn_g, w_router, w1, w3, w2, final_norm_g, _cores=None, _debug=(), _stop=None):
    f = lambda a: np.ascontiguousarray(np.asarray(a, dtype=np.float32))
    x, c, ctx, c_ctx = f(x), f(c), f(ctx), f(c_ctx)
    key = (tuple(sorted(_debug)), _stop)
    if key not in _NC_CACHE:
        _NC_CACHE[key] = build(_debug, _stop)
    g = _NC_CACHE[key]
    cst, cs, cls = _consts()
    gvec = np.concatenate([_fm(norm_mix_g[0], 32), _fm(norm_ffn_g[0], 32), _fm(final_norm_g, 32), _fm(b_gate[0], 64)], axis=1)
    shared = {
        "bmodT": _fm(b_mod[0], 192), "gvec": np.ascontiguousarray(gvec), "cst": cst,
        "w_mod": f(w_mod[0]), "w_in": f(w_in[0]), "w_fourier": f(w_fourier[0]), "w_na_out": f(w_na_out[0]),
        "w_out": f(w_out[0]), "w_router": f(w_router[0]), "w1": f(w1[0]), "w3": f(w3[0]), "w2": f(w2[0]),
        "dft_cs": cs, "dft_cls": cls, "nab": _bias_tables(f(na_rel_bias[0])),
    }
    cores = list(range(8)) if _cores is None else list(_cores)
    in_maps = []
    for b in cores:
        c2 = np.stack([c[b], c_ctx], axis=-1).reshape(32, 128, 2)
        c2T = np.ascontiguousarray(c2.transpose(1, 0, 2).reshape(128, 64))
        m = dict(shared)
        m.update({"x": x[b], "ctx": ctx[b], "c2T": c2T})
        in_maps.append(m)
    res = run_bass_kernel_spmd(g.nc, in_maps, core_ids=list(range(len(cores))))
    if _debug:
        return res.results
    return np.stack([r["out"] for r in res.results], axis=0).astype(np.float32)
```
